# Optimizing a Trainium2 kernel written in Bass

```python
import math
import jax, jax.numpy as jnp
from jax import lax
import numpy as np

D_MODEL = 2048
BATCH = 2
SEQ = 4096
DEPTH = 1

ATT_HEADS = 8
ATT_QK_DIM = 64
ATT_V_DIM = 128
ATT_WIDTH = ATT_HEADS * ATT_V_DIM
Q_BLOCK = 128
SG_GROUPS = 8
SG_DIM = 128
SG_WIDTH = SG_GROUPS * SG_DIM
SG_CHUNK = 128
MIX_WIDTH = ATT_WIDTH + SG_WIDTH
Q_OFF = 0
K_OFF = Q_OFF + ATT_HEADS * 2 * ATT_QK_DIM
V_OFF = K_OFF + ATT_HEADS * 2 * ATT_QK_DIM
U_OFF = V_OFF + ATT_WIDTH
G_OFF = U_OFF + SG_WIDTH
IN_WIDTH = G_OFF + SG_WIDTH
REL_BUCKETS = 32
REL_MAX_DIST = 128
N_GROUPS = 4
EXPERTS_PER_GROUP = 8
N_EXPERTS = N_GROUPS * EXPERTS_PER_GROUP
EXPERT_TOPK = 2
D_EXPERT = 256
DN_ALPHA = (2.0 * DEPTH) ** 0.25
DN_BETA = (8.0 * DEPTH) ** -0.25
LN_EPS = 1e-5

kernel_name = "hybrid_diffattn_gmlp_hmoe_deepnorm_encoder"


def _lambda_init(layer):
    return 0.8 - 0.6 * math.exp(-0.3 * layer)


def _layernorm(x, g, b):
    xf = x.astype(jnp.float32)
    mu = jnp.mean(xf, axis=-1, keepdims=True)
    var = jnp.mean(jnp.square(xf - mu), axis=-1, keepdims=True)
    return ((xf - mu) * lax.rsqrt(var + LN_EPS)).astype(x.dtype) * g + b


def _rel_bucket(rel):
    half = REL_BUCKETS // 2
    max_exact = half // 2
    ret = jnp.where(rel > 0, half, 0)
    n = jnp.abs(rel)
    nf = jnp.maximum(n, 1).astype(jnp.float32)
    large = max_exact + (jnp.log(nf / max_exact) / math.log(REL_MAX_DIST / max_exact)
                         * (half - max_exact)).astype(jnp.int32)
    large = jnp.minimum(large, half - 1)
    return ret + jnp.where(n < max_exact, n, large)


def _diff_attention(q, k, v, rel_bias, lam, subln_g, lam_init):
    B, S = q.shape[0], q.shape[1]
    nb = S // Q_BLOCK
    qb = (q * (ATT_QK_DIM ** -0.5)).reshape(B, nb, Q_BLOCK, ATT_HEADS, 2, ATT_QK_DIM)
    qb = qb.transpose(1, 0, 2, 3, 4, 5)
    kpos = jnp.arange(S, dtype=jnp.int32)

    def block(args):
        qblk, start = args
        qpos = start + jnp.arange(Q_BLOCK, dtype=jnp.int32)
        bias = rel_bias[_rel_bucket(kpos[None, :] - qpos[:, None])]
        bias = bias.transpose(2, 0, 1).astype(jnp.float32)
        logits = jnp.einsum('bqhmd,bkhmd->bmhqk', qblk, k).astype(jnp.float32) + bias[None, None]
        probs = jax.nn.softmax(logits, axis=-1)
        attn = probs[:, 0] - lam * probs[:, 1]
        return jnp.einsum('bhqk,bkhd->bqhd', attn.astype(v.dtype), v)

    starts = jnp.arange(nb, dtype=jnp.int32) * Q_BLOCK
    o = lax.map(block, (qb, starts))
    o = o.transpose(1, 0, 2, 3, 4).reshape(B, S, ATT_HEADS, ATT_V_DIM)
    of = o.astype(jnp.float32)
    of = of * lax.rsqrt(jnp.mean(jnp.square(of), axis=-1, keepdims=True) + LN_EPS)
    o = of.astype(v.dtype) * subln_g * (1.0 - lam_init)
    return o.reshape(B, S, ATT_WIDTH)


def _spatial_gating(u, vg, ln_g, ln_b, w_s, b_s):
    B, S = u.shape[0], u.shape[1]
    nc = S // SG_CHUNK
    v = vg.reshape(B, nc, SG_CHUNK, SG_GROUPS, SG_DIM)
    vn = _layernorm(v, ln_g, ln_b)
    mixed = jnp.einsum('gpq,bcqge->bcpge', w_s, vn) + b_s.T[:, :, None]
    return (u.reshape(B, nc, SG_CHUNK, SG_GROUPS, SG_DIM) * mixed).reshape(B, S, SG_WIDTH)


def _hier_moe(x, w_rg, b_rg, w_re, b_re, w_gate, w_up, w_down):
    B, S, D = x.shape
    t = x.reshape(B * S, D)
    g_logits = jnp.einsum('td,dg->tg', t, w_rg).astype(jnp.float32) + b_rg
    g_prob = jax.nn.softmax(g_logits, axis=-1)
    g_idx = jnp.argmax(g_logits, axis=-1)
    g_gate = jnp.take_along_axis(g_prob, g_idx[:, None], axis=-1)
    e_all = jnp.einsum('td,gde->tge', t, w_re).astype(jnp.float32) + b_re
    e_logits = jnp.take_along_axis(e_all, g_idx[:, None, None], axis=1)[:, 0]
    top_v, top_i = lax.top_k(e_logits, EXPERT_TOPK)
    top_w = jax.nn.softmax(top_v, axis=-1) * g_gate
    flat_idx = g_idx[:, None] * EXPERTS_PER_GROUP + top_i
    combine = jnp.sum(jax.nn.one_hot(flat_idx, N_EXPERTS, dtype=jnp.float32) * top_w[..., None], axis=1)
    h = jax.nn.silu(jnp.einsum('td,edf->tef', t, w_gate)) * jnp.einsum('td,edf->tef', t, w_up)
    h = h * combine[:, :, None].astype(t.dtype)
    return jnp.einsum('tef,efd->td', h, w_down).reshape(B, S, D)


def setup_inputs(seed: int = 0) -> dict:
    key = jax.random.key(seed)
    ks = jax.random.split(key, 24)
    f32 = jnp.float32
    D = D_MODEL
    nrm = lambda k, s, sc: jax.random.normal(k, s, f32) * sc
    col_scale = jnp.ones((IN_WIDTH,), f32).at[V_OFF:V_OFF + ATT_WIDTH].set(DN_BETA)
    return {
        "x": jax.random.normal(ks[0], (BATCH, SEQ, D), f32),
        "w_in": nrm(ks[1], (DEPTH, D, IN_WIDTH), D ** -0.5) * col_scale,
        "w_out": nrm(ks[2], (DEPTH, MIX_WIDTH, D), MIX_WIDTH ** -0.5 * DN_BETA),
        "ln1_g": 1.0 + nrm(ks[3], (DEPTH, D), 0.01),
        "ln1_b": nrm(ks[4], (DEPTH, D), 0.01),
        "ln2_g": 1.0 + nrm(ks[5], (DEPTH, D), 0.01),
        "ln2_b": nrm(ks[6], (DEPTH, D), 0.01),
        "rel_bias": nrm(ks[7], (REL_BUCKETS, ATT_HEADS), 0.5),
        "lam_q1": nrm(ks[8], (DEPTH, ATT_QK_DIM), 0.1),
        "lam_k1": nrm(ks[9], (DEPTH, ATT_QK_DIM), 0.1),
        "lam_q2": nrm(ks[10], (DEPTH, ATT_QK_DIM), 0.1),
        "lam_k2": nrm(ks[11], (DEPTH, ATT_QK_DIM), 0.1),
        "subln_g": 1.0 + nrm(ks[12], (DEPTH, ATT_V_DIM), 0.01),
        "sg_ln_g": 1.0 + nrm(ks[13], (DEPTH, SG_GROUPS, SG_DIM), 0.01),
        "sg_ln_b": nrm(ks[14], (DEPTH, SG_GROUPS, SG_DIM), 0.01),
        "sg_w": nrm(ks[15], (DEPTH, SG_GROUPS, SG_CHUNK, SG_CHUNK), SG_CHUNK ** -0.5),
        "sg_b": 1.0 + nrm(ks[16], (DEPTH, SG_GROUPS, SG_CHUNK), 0.1),
        "w_router_group": nrm(ks[17], (DEPTH, D, N_GROUPS), D ** -0.5),
        "b_router_group": nrm(ks[18], (DEPTH, N_GROUPS), 0.01),
        "w_router_expert": nrm(ks[19], (DEPTH, N_GROUPS, D, EXPERTS_PER_GROUP), D ** -0.5),
        "b_router_expert": nrm(ks[20], (DEPTH, N_GROUPS, EXPERTS_PER_GROUP), 0.01),
        "w_exp_gate": nrm(ks[21], (DEPTH, N_EXPERTS, D, D_EXPERT), D ** -0.5),
        "w_exp_up": nrm(ks[22], (DEPTH, N_EXPERTS, D, D_EXPERT), D ** -0.5),
        "w_exp_down": nrm(ks[23], (DEPTH, N_EXPERTS, D_EXPERT, D), D_EXPERT ** -0.5 * DN_BETA),
    }


def reference(x, w_in, w_out, ln1_g, ln1_b, ln2_g, ln2_b, rel_bias, lam_q1, lam_k1, lam_q2, lam_k2,
              subln_g, sg_ln_g, sg_ln_b, sg_w, sg_b, w_router_group, b_router_group,
              w_router_expert, b_router_expert, w_exp_gate, w_exp_up, w_exp_down):
    B, S = x.shape[0], x.shape[1]
    h = x
    for l in range(DEPTH):
        lam_init = _lambda_init(l)
        proj = jnp.einsum('bsd,de->bse', h, w_in[l])
        q = proj[..., Q_OFF:K_OFF].reshape(B, S, ATT_HEADS, 2, ATT_QK_DIM)
        k = proj[..., K_OFF:V_OFF].reshape(B, S, ATT_HEADS, 2, ATT_QK_DIM)
        v = proj[..., V_OFF:U_OFF].reshape(B, S, ATT_HEADS, ATT_V_DIM)
        uv = jax.nn.gelu(proj[..., U_OFF:IN_WIDTH])
        u, vg = uv[..., :SG_WIDTH], uv[..., SG_WIDTH:]
        lam = (jnp.exp(jnp.sum(lam_q1[l].astype(jnp.float32) * lam_k1[l].astype(jnp.float32)))
               - jnp.exp(jnp.sum(lam_q2[l].astype(jnp.float32) * lam_k2[l].astype(jnp.float32)))
               + lam_init)
        a = _diff_attention(q, k, v, rel_bias, lam, subln_g[l], lam_init)
        s = _spatial_gating(u, vg, sg_ln_g[l], sg_ln_b[l], sg_w[l], sg_b[l])
        mix = jnp.einsum('bse,ed->bsd', jnp.concatenate([a, s], axis=-1), w_out[l])
        h = _layernorm(DN_ALPHA * h + mix, ln1_g[l], ln1_b[l])
        ffn = _hier_moe(h, w_router_group[l], b_router_group[l], w_router_expert[l], b_router_expert[l],
                        w_exp_gate[l], w_exp_up[l], w_exp_down[l])
        h = _layernorm(DN_ALPHA * h + ffn, ln2_g[l], ln2_b[l])
    return h
```

```python
import contextlib
import math
import os
import numpy as np
import concourse.bass as bass
import concourse.mybir as mybir
from concourse.bass_utils import run_bass_kernel_spmd

F32 = mybir.dt.float32
BF16 = mybir.dt.bfloat16
AF = mybir.ActivationFunctionType
ALU = mybir.AluOpType
AX = mybir.AxisListType

D = 2048
SEQ = 4096
NT = 1024
ALPHA = 2.0 ** 0.25
EPS = 1e-5
LAM_INIT = 0.8 - 0.6 * math.exp(0.0)
NTYPES = 9
MOE_SPARSE = True


def _name_of(k):
    return k[0] if isinstance(k, tuple) else k


class Sched:
    ENGS = ("pe", "act", "dve", "pool", "sp")

    def __init__(self, nc, es, n_dma_sems=48, same_engine_waits=("act", "dve", "pool")):
        self.nc = nc
        self.ops = {e: [] for e in self.ENGS}
        self.count = {e: 0 for e in self.ENGS}
        self.sem = {e: es.enter_context(nc.semaphore("prog_" + e)) for e in self.ENGS}
        self.dsems = [es.enter_context(nc.semaphore("dma%d" % i)) for i in range(n_dma_sems)]
        self.dval = [0] * n_dma_sems
        self.dnext = 0
        self.waited = {e: {} for e in self.ENGS}
        self.last_w = {}
        self.readers = {}
        self.pending = {}
        self.same = set(same_engine_waits)

    def alias(self, newname, oldnames):
        toks = {}
        for k, t in self.last_w.items():
            if t is not None and _name_of(k) in oldnames:
                toks[t[0]] = max(toks.get(t[0], 0), t[1])
        for k, ts in self.readers.items():
            if _name_of(k) in oldnames:
                for t in ts:
                    toks[t[0]] = max(toks.get(t[0], 0), t[1])
        self.pending[newname] = list(toks.items())

    def _init_key(self, k):
        if k not in self.last_w and k not in self.readers:
            self.last_w[k] = None
            self.readers[k] = list(self.pending.get(_name_of(k), []))

    def _deps(self, eng, reads, writes):
        toks = []
        for k in reads:
            self._init_key(k)
            t = self.last_w.get(k)
            if t is not None:
                toks.append(t)
        for k in writes:
            self._init_key(k)
            t = self.last_w.get(k)
            if t is not None:
                toks.append(t)
            toks.extend(self.readers.get(k, ()))
        need = {}
        for (s, v) in toks:
            if isinstance(s, str) and s == eng and eng not in self.same:
                continue
            if v > need.get(s, 0):
                need[s] = v
        out = []
        for s, v in need.items():
            if self.waited[eng].get(s, 0) >= v:
                continue
            self.waited[eng][s] = v
            out.append((s, v))
        return out

    def _record(self, tok, reads, writes):
        for k in writes:
            self.last_w[k] = tok
            self.readers[k] = []
        for k in reads:
            if k in writes:
                continue
            self.readers.setdefault(k, []).append(tok)

    def op(self, eng, fn, reads=(), writes=()):
        waits = self._deps(eng, reads, writes)
        self.count[eng] += 1
        tok = (eng, self.count[eng])
        self.ops[eng].append(("op", fn, waits))
        self._record(tok, reads, writes)
        return tok

    def dma(self, queue, out, in_, reads=(), writes=(), **kw):
        i = self.dnext
        self.dnext = (self.dnext + 1) % len(self.dsems)
        waits = self._deps(queue, reads, writes)
        prev = self.dval[i]
        if prev > 0 and self.waited[queue].get(i, 0) < prev:
            self.waited[queue][i] = prev
            waits.append((i, prev))
        self.dval[i] += 16
        tok = (i, self.dval[i])
        self.ops[queue].append(("dma", (out, in_, kw), waits, i))
        self._record(tok, reads, writes)
        return tok

    def dma_fn(self, queue, fn, reads=(), writes=()):
        i = self.dnext
        self.dnext = (self.dnext + 1) % len(self.dsems)
        waits = self._deps(queue, reads, writes)
        prev = self.dval[i]
        if prev > 0 and self.waited[queue].get(i, 0) < prev:
            self.waited[queue][i] = prev
            waits.append((i, prev))
        self.dval[i] += 16
        tok = (i, self.dval[i])
        self.ops[queue].append(("dmaf", fn, waits, i))
        self._record(tok, reads, writes)
        return tok

    def _semof(self, s):
        return self.sem[s] if isinstance(s, str) else self.dsems[s]

    def emit(self, final_wait_keys=()):
        nc = self.nc
        fw = []
        for k in final_wait_keys:
            t = self.last_w.get(k)
            if t is not None:
                fw.append(t)
        sched = self

        def run(eng_name, eng):
            for item in sched.ops[eng_name]:
                if item[0] == "op":
                    _, fn, waits = item
                    for (s, v) in waits:
                        eng.wait_ge(sched._semof(s), v)
                    ins = fn(eng)
                    ins.then_inc(sched.sem[eng_name], 1)
                elif item[0] == "dmaf":
                    _, fn, waits, i = item
                    for (s, v) in waits:
                        eng.wait_ge(sched._semof(s), v)
                    fn(eng).then_inc(sched.dsems[i], 16)
                else:
                    _, (out, in_, kw), waits, i = item
                    for (s, v) in waits:
                        eng.wait_ge(sched._semof(s), v)
                    eng.dma_start(out=out, in_=in_, **kw).then_inc(sched.dsems[i], 16)
            if eng_name == "sp":
                for (s, v) in fw:
                    eng.wait_ge(sched._semof(s), v)

        with nc.Block() as block:
            @block.tensor
            def _(e):
                run("pe", e)

            @block.scalar
            def _(e):
                run("act", e)

            @block.vector
            def _(e):
                run("dve", e)

            @block.gpsimd
            def _(e):
                run("pool", e)

            @block.sync
            def _(e):
                run("sp", e)


class Arena:
    def __init__(self, S, ap):
        self.S = S
        self.ap = ap
        self.live = []

    def alloc(self, name, at, dtype, free_shape):
        esz = 2 if dtype == BF16 else 4
        nel = int(np.prod(free_shape))
        nbytes = nel * esz
        start, end = at, at + nbytes
        assert start % 4 == 0 and end <= self.ap.shape[1] * 2, (name, start, end)
        olds = [n for (s, e, n) in self.live if s < end and start < e]
        self.live.append((start, end, name))
        if olds:
            self.S.alias(name, set(olds))
        v = self.ap[:, start // 2:end // 2]
        if dtype != BF16:
            v = v.bitcast(dtype)
        if len(free_shape) == 2:
            v = v.rearrange("p (a b) -> p a b", a=free_shape[0])
        elif len(free_shape) == 3:
            v = v.rearrange("p (a b c) -> p a b c", a=free_shape[0], b=free_shape[1])
        return v


class Rot:
    def __init__(self, items):
        self.items = list(items)
        self.i = 0

    def next(self):
        v = self.items[self.i % len(self.items)]
        self.i += 1
        return v


def build(stage="D"):
    nc = bass.Bass("TRN2", target_bir_lowering=False)

    def din(name, shape, dtype=F32):
        return nc.dram_tensor(name, list(shape), dtype, kind="ExternalInput").ap()

    def dout(name, shape, dtype=F32):
        return nc.dram_tensor(name, list(shape), dtype, kind="ExternalOutput").ap()

    xT_all = din("xT_all", [16, 128, 16 * 256])
    w_kv = din("w_kv", [4, 2, 128, 16 * 256])
    xT_own = din("xT_own", [128, 16 * NT])
    x_own = din("x_own", [NT, D])
    w_inA = din("w_inA", [6, 128, 16 * 512])
    w_outT = din("w_outT", [4, 128, 16 * 512])
    w_gu = din("w_gu", [32, 128, 16 * 512])
    w_dn = din("w_dn", [32, 128, 2 * 2048])
    wr_d = din("wr_cat", [D, 36])
    brB_d = din("br_b", [128, 36])
    ln1g_d = din("ln1g_b", [128, D])
    ln1b_d = din("ln1b_b", [128, D])
    ln2g_d = din("ln2g_b", [128, D])
    ln2b_d = din("ln2b_b", [128, D])
    relb_d = din("rel_bias", [32, 8])
    lamp_d = din("lamp_b", [128, 256])
    subg_d = din("subg_b", [128, 128])
    sglng_d = din("sglng_b", [128, 1024])
    sglnb_d = din("sglnb_b", [128, 1024])
    sgwT_d = din("sg_wT", [8, 128, 128])
    sgb_d = din("sg_b_row", [1, 1024])
    E1_d = din("E1", [32, NTYPES * 256])
    E2_d = din("E2", [32, 26])
    ident_d = din("ident", [128, 128])
    uscr = nc.dram_tensor("uscr", [8, NTYPES * 256], F32, kind="Internal").ap()
    NSL = 48
    if MOE_SPARSE:
        tri_d = din("tri", [128, 128])
        iota_d = din("iota_p", [128, 1])
        Xslots = nc.dram_tensor("Xslots", [NSL * 128, D], BF16, kind="Internal").ap()
        Yslots = nc.dram_tensor("Yslots", [NSL * 128, D], BF16, kind="Internal").ap()

    with contextlib.ExitStack() as es:
        S = Sched(nc, es)

        def sb(name, shape, dtype):
            return es.enter_context(nc.sbuf_tensor(name, list(shape), dtype))

        psp = [es.enter_context(nc.psum_tensor("psp%d" % i, [128, 1024], F32)) for i in range(4)]
        ps = [psp[i // 2][:, (i % 2) * 512:(i % 2 + 1) * 512] for i in range(8)]
        PK = ["ps%d" % i for i in range(8)]

        ident_f = sb("ident_f", [128, 128], F32)
        ident_b = sb("ident_b", [128, 128], BF16)
        ones_f = sb("ones_f", [32, 128], F32)
        ones_b = sb("ones_b", [1, 128], BF16)
        lamp = sb("lamp", [128, 256], F32)
        lamw = sb("lamw", [128, 8], F32)
        subg = sb("subg", [128, 128], F32)
        relb = sb("relb", [32, 8], F32)
        E2s = sb("E2s", [32, 26], F32)
        rE2 = sb("rE2", [32, 8, 26], F32)
        cbB = sb("cbB", [128, 8, 26], F32)
        sgb_row = sb("sgb_row", [1, 1024], BF16)
        comb = sb("comb", [128, 8, 32], F32)
        OH = sb("OH", [128, 8, 64], F32)
        W12 = sb("W12", [128, 8, 2], F32)
        QT = sb("QT", [128, 8 * 1024], BF16)
        asT = sb("asT", [128, 16, 1024], BF16)
        arena_t = sb("arena", [128, 65536], BF16)
        AR = Arena(S, arena_t[:, :])
        QTv = QT[:, :].rearrange("p (h t) -> p h t", h=8)
        QA = Arena(S, QT[:, :])
        QA.live.append((0, 16384, "QT"))

        S.dma("sp", ident_f[:], ident_d, writes=["ident_f"])
        S.dma("pool", ident_b[:], ident_d, writes=["ident_b"])
        S.dma("sp", lamp[:], lamp_d, writes=["lamp"])
        S.dma("sp", subg[:], subg_d, writes=["subg"])
        S.dma("sp", relb[:], relb_d, writes=["relb"])
        S.dma("sp", E2s[:], E2_d, writes=["E2s"])
        S.dma("pool", sgb_row[:], sgb_d, writes=["sgb_row"])
        S.op("dve", lambda e: e.memset(ones_f[:], 1.0), writes=["ones_f"])
        S.op("dve", lambda e: e.memset(ones_b[:], 1.0), writes=["ones_b"])
        mhalf = sb("mhalf", [128, 8], F32)
        S.op("dve", lambda e: e.memset(mhalf[:], -0.5), writes=["mhalf"])
        lp = lamp[:, :].rearrange("p (a b) -> p a b", a=4)
        prod = sb("lamprod", [128, 2, 64], F32)
        S.op("dve", lambda e: e.tensor_tensor(prod[:, 0, :], lp[:, 0, :], lp[:, 1, :], ALU.mult),
             reads=["lamp"], writes=["lamprod0"])
        S.op("dve", lambda e: e.tensor_tensor(prod[:, 1, :], lp[:, 2, :], lp[:, 3, :], ALU.mult),
             reads=["lamp"], writes=["lamprod1"])
        S.op("dve", lambda e: e.reduce_sum(lamw[:, 0:2], prod[:, :, :], AX.X),
             reads=["lamprod0", "lamprod1"], writes=["lamw01"])
        S.op("act", lambda e: e.activation(lamw[:, 2:4], lamw[:, 0:2], AF.Exp), reads=["lamw01"], writes=["lamw23"])
        S.op("dve", lambda e: e.tensor_tensor(lamw[:, 4:5], lamw[:, 2:3], lamw[:, 3:4], ALU.subtract),
             reads=["lamw23"], writes=["lamw4"])
        S.op("dve", lambda e: e.tensor_scalar(lamw[:, 5:6], lamw[:, 4:5], -1.0, -LAM_INIT, ALU.mult, ALU.add),
             reads=["lamw4"], writes=["nlam"])
        S.op("dve", lambda e: e.tensor_scalar(subg[:], subg[:], 1.0 - LAM_INIT, None, ALU.mult),
             reads=["subg"], writes=["subg"])
        nlam = lamw[:, 5:6]

        biasT = AR.alloc("biasT", 0, BF16, [8, NTYPES, 128])
        E1s = AR.alloc("E1s", 83968, F32, [NTYPES * 256])
        u_sb = AR.alloc("u_sb", 93184, F32, [NTYPES * 256])
        S.dma("sp", E1s[0:32, :], E1_d, writes=["E1s"])
        ncol = NTYPES * 256
        for ci, c0 in enumerate(range(0, ncol, 512)):
            w = min(512, ncol - c0)
            bk = ci % 4

            def f(e, c0=c0, w=w, bk=bk):
                return e.matmul(ps[bk][0:8, 0:w], relb[:, :], E1s[0:32, c0:c0 + w], start=True, stop=True)
            S.op("pe", f, reads=["relb", "E1s"], writes=[PK[bk]])
            S.op("act", lambda e, c0=c0, w=w, bk=bk: e.activation(u_sb[0:8, c0:c0 + w], ps[bk][0:8, 0:w], AF.Copy, scale=8.0),
                 reads=[PK[bk]], writes=[("u_sb", ci)])
        S.dma("sp", uscr, u_sb[0:8, :], reads=[("u_sb", ci) for ci in range((ncol + 511) // 512)], writes=["uscr"])
        for h in range(8):
            src = bass.AP(tensor=uscr.tensor, offset=h * ncol, ap=[[1, 128], [256, NTYPES], [1, 128]])
            S.dma("pool", biasT[:, h, :, :], src, reads=["uscr"], writes=[("biasT", h)])
        for h in range(8):
            S.op("dve", lambda e, h=h: e.tensor_scalar(rE2[:, h, :], E2s[:, :], relb[:, h:h + 1], None, ALU.mult),
                 reads=["relb", "E2s"], writes=[("rE2", h)])
        S.op("pe", lambda e: e.matmul(ps[4][:, 0:208], ones_f[:, :], rE2[:, :, :].rearrange("p a b -> p (a b)"), start=True, stop=True),
             reads=["ones_f"] + [("rE2", h) for h in range(8)], writes=[PK[4]])
        S.op("dve", lambda e: e.tensor_copy(cbB[:, :, :].rearrange("p a b -> p (a b)"), ps[4][:, 0:208]), reads=[PK[4]], writes=["cbB"])

        if MOE_SPARSE:
            zt = asT[:, 0:2, :].rearrange("p a b -> p (a b)")
            ztk = [("asT", h_, t_) for h_ in range(2) for t_ in range(8)]
            S.op("dve", lambda e: e.memset(zt, 0.0), writes=ztk)
            for j in range(48):
                S.dma("sp", Xslots[j * 128:(j + 1) * 128, :], zt, reads=ztk, writes=["Xzero"])

        xTo = AR.alloc("xTo", 18432, BF16, [16, 1024])
        wA = [AR.alloc("wA0", 51200, BF16, [16, 512]), AR.alloc("wA1", 67584, BF16, [16, 512])]
        uT = AR.alloc("uT", 83968, BF16, [8, 1024])
        lnAg = AR.alloc("lnAg", 100352, F32, [1024])
        lnAb = AR.alloc("lnAb", 104448, F32, [1024])
        sgwT = AR.alloc("sgwT", 108544, BF16, [8, 128])
        vgt = [AR.alloc("vgt0", 110592, F32, [512]), AR.alloc("vgt1", 112640, F32, [512])]
        sqt = AR.alloc("sqt", 114688, F32, [512])
        vnf = AR.alloc("vnf", 116736, F32, [512])
        vnb = [AR.alloc("vnb0", 118784, BF16, [4, 128]), AR.alloc("vnb1", 119808, BF16, [4, 128])]
        stA = AR.alloc("stA", 120832, F32, [2, 32])

        S.dma("pool", xTo.rearrange("p a b -> p (a b)"), xT_own, writes=["xTo"])
        S.dma("sp", lnAg, sglng_d, writes=["lnAg"])
        S.dma("sp", lnAb, sglnb_d, writes=["lnAb"])
        S.dma("pool", sgwT, sgwT_d.rearrange("g q p -> q g p"), writes=["sgwT"])

        rot4 = Rot([0, 1, 2, 3])
        evq = Rot(["act", "dve"])
        sqt2 = [sqt, AR.alloc("sqt1", 121344, F32, [512])]
        vnf2 = [vnf, AR.alloc("vnf1", 123392, F32, [512])]
        gchains = []
        gcount = [0]

        def interleave(chains, width):
            it = iter(chains)
            active = []
            for _ in range(width):
                c = next(it, None)
                if c is not None:
                    active.append(list(c))
            while active:
                for c in list(active):
                    c.pop(0)()
                    if not c:
                        active.remove(c)
                        n = next(it, None)
                        if n is not None:
                            active.append(list(n))

        def g_chain(bi, i, tt, wt, wkey):
            vb = gcount[0] % 2
            gcount[0] += 1
            bkc = [None]
            vg = vgt[vb]
            vgk = "vgt%d" % vb
            sq = sqt2[vb]
            vf = vnf2[vb]
            sfx = "_%d" % vb
            vg3 = vg.rearrange("p (a b) -> p a b", a=4)
            sq3 = sq.rearrange("p (a b) -> p a b", a=4)
            s1, s2, mean, m2 = stA[:, vb, 0:4], stA[:, vb, 4:8], stA[:, vb, 8:12], stA[:, vb, 12:16]
            var, rstd = stA[:, vb, 16:20], stA[:, vb, 20:24]
            vn = vnb[vb]
            vnk = "vnb%d" % vb
            sbk = 4 + vb
            ps3 = ps[sbk][:, :].rearrange("p (a b) -> p a b", a=4)
            st = []

            def s_mm():
                bk = rot4.next()
                bkc[0] = bk

                def f(e):
                    for c in range(16):
                        ins = e.matmul(ps[bk][:, :], xTo[:, c, tt * 128:(tt + 1) * 128], wt[:, c, :],
                                       start=(c == 0), stop=(c == 15))
                    return ins
                S.op("pe", f, reads=[wkey, "xTo"], writes=[PK[bk]])
                S.op("act", lambda e: e.activation(vg, ps[bk][:, :], AF.Gelu_apprx_tanh), reads=[PK[bk]], writes=[vgk])
            st.append(s_mm)
            st.append(lambda: S.op("dve", lambda e: e.reduce_sum(s1, vg3, AX.X), reads=[vgk], writes=["s1" + sfx]))
            st.append(lambda: S.op("dve", lambda e: e.tensor_tensor(sq, vg, vg, ALU.mult), reads=[vgk], writes=["sqt" + sfx]))
            st.append(lambda: S.op("dve", lambda e: e.reduce_sum(s2, sq3, AX.X), reads=["sqt" + sfx], writes=["s2" + sfx]))
            st.append(lambda: S.op("dve", lambda e: e.tensor_scalar(mean, s1, 1.0 / 128, None, ALU.mult), reads=["s1" + sfx], writes=["mean" + sfx]))
            st.append(lambda: S.op("dve", lambda e: e.tensor_tensor(m2, mean, mean, ALU.mult), reads=["mean" + sfx], writes=["m2" + sfx]))
            st.append(lambda: S.op("dve", lambda e: e.scalar_tensor_tensor(var, s2, 1.0 / 128, m2, ALU.mult, ALU.subtract),
                                   reads=["s2" + sfx, "m2" + sfx], writes=["var" + sfx]))
            st.append(lambda: S.op("dve", lambda e: e.tensor_scalar(var, var, EPS, None, ALU.add), reads=["var" + sfx], writes=["var" + sfx]))
            st.append(lambda: S.op("pool", lambda e: e.tensor_tensor(rstd, var, mhalf[:, 0:4], ALU.pow), reads=["var" + sfx, "mhalf"], writes=["rstd" + sfx]))
            for gg in range(4):
                st.append(lambda gg=gg: S.op("dve", lambda e: e.tensor_scalar(
                    vf[:, gg * 128:(gg + 1) * 128], vg[:, gg * 128:(gg + 1) * 128],
                    mean[:, gg:gg + 1], rstd[:, gg:gg + 1], ALU.subtract, ALU.mult),
                    reads=[vgk, "mean" + sfx, "rstd" + sfx], writes=[("vnf" + sfx, gg)]))
            st.append(lambda: S.op("dve", lambda e: e.tensor_tensor(vf, vf, lnAg[:, i * 512:(i + 1) * 512], ALU.mult),
                                   reads=[("vnf" + sfx, g_) for g_ in range(4)] + ["lnAg"], writes=["vnf2" + sfx]))
            st.append(lambda: S.op("dve", lambda e: e.tensor_tensor(vn.rearrange("p a b -> p (a b)"), vf, lnAb[:, i * 512:(i + 1) * 512], ALU.add),
                                   reads=["vnf2" + sfx, "lnAb"], writes=[vnk] + [("vnf" + sfx, g_) for g_ in range(4)]))

            def s_sp():
                def fs(e):
                    for gg in range(4):
                        g = i * 4 + gg
                        e.matmul(ps3[:, gg, :], vn[:, gg, :], sgwT[:, g, :], start=True, stop=False)
                        ins = e.matmul(ps3[:, gg, :], ones_b[0:1, :], sgb_row[0:1, g * 128:(g + 1) * 128],
                                       start=False, stop=True)
                    return ins
                S.op("pe", fs, reads=[vnk, "sgwT", "ones_b", "sgb_row"], writes=[PK[sbk]])
                dst = asT[:, 8 + i * 4:8 + (i + 1) * 4, tt * 128:(tt + 1) * 128]
                usrc = uT[:, i * 4:(i + 1) * 4, tt * 128:(tt + 1) * 128]
                S.op("dve", lambda e: e.tensor_tensor(dst, usrc, ps3, ALU.mult),
                     reads=[PK[sbk]] + [("uT", i * 4 + g_, tt // 4) for g_ in range(4)],
                     writes=[("asT", 8 + i * 4 + g_, tt) for g_ in range(4)])
            st.append(s_sp)
            return st

        blocks = [("Q", 0, 0), ("Q", 1, 512), ("U", 0, 3072), ("U", 1, 3584), ("G", 0, 4096), ("G", 1, 4608)]
        kA = 0
        for bi, (kind, i, c0) in enumerate(blocks):
            buf = bi % 2
            wkey = "wA%d" % buf
            wt = wA[buf]
            S.dma("pool", wt.rearrange("p a b -> p (a b)"), w_inA[bi], writes=[wkey])
            if kind in ("Q", "U"):
                for hh in range(4):
                    h = i * 4 + hh
                    for th in range(2):
                        bk = rot4.next()

                        def f(e, wt=wt, hh=hh, th=th, bk=bk):
                            for c in range(16):
                                ins = e.matmul(ps[bk][:, :], wt[:, c, hh * 128:(hh + 1) * 128],
                                               xTo[:, c, th * 512:(th + 1) * 512], start=(c == 0), stop=(c == 15))
                            return ins
                        S.op("pe", f, reads=[wkey, "xTo"], writes=[PK[bk]])
                        if kind == "Q":
                            q = evq.next()
                            dst = QTv[:, h, th * 512:(th + 1) * 512]
                            if q == "act":
                                S.op("act", lambda e, dst=dst, bk=bk: e.activation(dst, ps[bk][:, :], AF.Copy),
                                     reads=[PK[bk]], writes=[("QT", h, th)])
                            else:
                                S.op("dve", lambda e, dst=dst, bk=bk: e.tensor_copy(dst, ps[bk][:, :]),
                                     reads=[PK[bk]], writes=[("QT", h, th)])
                        else:
                            dst = uT[:, h, th * 512:(th + 1) * 512]
                            S.op("act", lambda e, dst=dst, bk=bk: e.activation(dst, ps[bk][:, :], AF.Gelu_apprx_tanh),
                                 reads=[PK[bk]], writes=[("uT", h, th)])
            else:
                for tt in range(8):
                    gchains.append(g_chain(bi, i, tt, wt, wkey))
        interleave(gchains, 2)

        outs = []

        def dump(name, src_ap, shape, dtype, reads):
            o = dout(name, shape, dtype)
            S.dma("sp", o, src_ap, reads=reads, writes=[name])
            outs.append(name)

        if stage == "A":
            dump("dbg_sT", asT[:, 8:16, :], [128, 8, 1024], BF16,
                 [("asT", 8 + g, tt) for g in range(8) for tt in range(8)])
            dump("dbg_QT", QTv, [128, 8, 1024], BF16, [("QT", h, th) for h in range(8) for th in range(2)])
            dump("dbg_bias", biasT, [128, 8, NTYPES, 128], BF16, [("biasT", h) for h in range(8)])
            dump("dbg_cb", cbB[:, :, :], [128, 8, 26], F32, ["cbB"])
            dump("dbg_lam", lamw[:, :], [128, 8], F32, ["nlam"])
            S.emit(final_wait_keys=outs)
            return nc

        KT = [AR.alloc("KT0", 18432, BF16, [2, 4096]), AR.alloc("KT1", 34816, BF16, [2, 4096])]
        V = [AR.alloc("V0", 51200, BF16, [32, 2, 130]), AR.alloc("V1", 67840, BF16, [32, 2, 130])]
        wk = AR.alloc("wk", 84480, BF16, [16, 256])
        wv = AR.alloc("wv", 92672, BF16, [16, 256])
        xtb = [AR.alloc("xt0", 100864, BF16, [16, 256]), AR.alloc("xt1", 109056, BF16, [16, 256])]
        PT = [AR.alloc("PT%d" % k, 117248 + k * 1024, BF16, [2, 256]) for k in range(3)]
        accS = [AR.alloc("accS%d" % k, 120320 + k * 2080, F32, [4, 130]) for k in range(2)]
        o_f = AR.alloc("o_f", 124480, F32, [2, 128])
        sq_f = AR.alloc("sq_f", 125504, F32, [2, 128])
        a_bf = [AR.alloc("a_bf%d" % k, 126528 + k * 512, BF16, [2, 128]) for k in range(2)]
        stB = AR.alloc("stB", 127552, F32, [64])

        for vb in range(2):
            S.op("dve", lambda e, vb=vb: e.memset(V[vb][:, :, :, 128:129], 1.0), writes=["Vones%d" % vb])

        def near_tiles(qg4):
            return [j for j in range(max(0, 2 * qg4 - 1), 2 * qg4 + 3)] + ([31] if qg4 == 0 else [])

        def btype(i, j):
            if j <= 7:
                d = j - i
                return 0 if d == 0 else 1 if d == 1 else 2 if d == -1 else 3 if d >= 2 else 4
            if j == 8:
                return 5 if i == 7 else 6
            assert j == 31
            return 7 if i == 0 else 8

        def far_col(qg4, j):
            if j <= 7:
                return 25 if j > 2 * qg4 + 1 else 24
            return j - 8

        kvstate = {"x": 0}

        def kv_chunks(hg, banks, evs):
            vb = hg % 2
            ktk, vk = "KT%d" % vb, "V%d" % vb
            chunks = []

            def xdma(T):
                xb = (hg * 16 + T) % 2
                S.dma("pool", xtb[xb].rearrange("p a b -> p (a b)"), xT_all[T], writes=["xt%d" % xb])

            def mk(T, kind, idx):
                stt = {}
                xb = (hg * 16 + T) % 2
                xt = xtb[xb]
                xk = "xt%d" % xb

                def quarter(qi):
                    def run():
                        if qi == 0:
                            if T == 0 and kind == "K" and idx == 0:
                                S.dma("pool", wk.rearrange("p a b -> p (a b)"), w_kv[hg, 0], writes=["wk"])
                                S.dma("pool", wv.rearrange("p a b -> p (a b)"), w_kv[hg, 1], writes=["wv"])
                                xdma(0)
                            if kind == "K" and idx == 0 and T + 1 < 16:
                                xdma(T + 1)
                            stt["bk"] = banks.next()
                            stt["q"] = evs.next()
                        bk = stt["bk"]
                        c0 = qi * 4
                        if kind == "K":
                            def f(e):
                                for c in range(c0, c0 + 4):
                                    ins = e.matmul(ps[bk][:, 0:256], wk[:, c, idx * 128:(idx + 1) * 128], xt[:, c, :],
                                                   start=(c == 0), stop=(c == 15))
                                return ins
                            S.op("pe", f, reads=["wk", xk], writes=[PK[bk]])
                        else:
                            def f(e):
                                for c in range(c0, c0 + 4):
                                    ins = e.matmul(ps[bk][:, 0:256], xt[:, c, idx * 128:(idx + 1) * 128], wv[:, c, :],
                                                   start=(c == 0), stop=(c == 15))
                                return ins
                            S.op("pe", f, reads=["wv", xk], writes=[PK[bk]])
                        if qi == 3:
                            if kind == "K":
                                dst = KT[vb][:, idx, T * 256:(T + 1) * 256]
                                src_ = ps[bk][:, 0:256]
                                wkeys = [(ktk, idx, T // 2)]
                            else:
                                j = T * 2 + idx
                                dst = V[vb][:, j, :, 0:128]
                                src_ = ps[bk][:, 0:256].rearrange("p (a b) -> p a b", a=2)
                                wkeys = [(vk, j)]
                            if stt["q"] == "act":
                                S.op("act", lambda e: e.activation(dst, src_, AF.Copy), reads=[PK[bk], "Vones%d" % vb], writes=wkeys)
                            else:
                                S.op("dve", lambda e: e.tensor_copy(dst, src_), reads=[PK[bk], "Vones%d" % vb], writes=wkeys)
                    return run
                return [quarter(qi) for qi in range(4)]
            for T in range(16):
                for hl in range(2):
                    chunks.extend(mk(T, "K", hl))
                for st in range(2):
                    chunks.extend(mk(T, "V", st))
            return chunks

        for ch in kv_chunks(0, Rot([0, 1, 2, 3, 4, 5, 6, 7]), Rot(["act", "dve"])):
            ch()

        ps6b = ps[6][:, :].bitcast(BF16)
        stepno = 0
        grp = 0
        deferred = []

        def accv(idx):
            return (4, idx * 130) if idx < 3 else (5, 0)

        for hg in range(4):
            vb = hg % 2
            ktk, vk = "KT%d" % vb, "V%d" % vb
            pend = kv_chunks(hg + 1, Rot([7]), Rot(["dve"])) if hg < 3 else []
            for hl in range(2):
                h = hg * 2 + hl
                for qg4 in range(4):
                    near = near_tiles(qg4)

                    def pv(j, hl=hl, vb=vb, vk=vk):
                        pb = j % 3

                        def f(e):
                            for idx in range(4):
                                m, ib = divmod(idx, 2)
                                bkk, col = accv(idx)
                                ins = e.matmul(ps[bkk][:, col:col + 129], PT[pb][:, m, ib * 128:(ib + 1) * 128],
                                               V[vb][:, j, hl, 0:129], start=(j == 0 and idx in (0, 3)), stop=(j == 31),
                                               skip_group_check=True)
                            return ins
                        S.op("pe", f, reads=["PT%d" % pb, (vk, j)], writes=[PK[4], PK[5]])

                    for j in range(32):
                        sk = stepno % 2
                        stepno += 1
                        b0, b1 = 2 * sk, 2 * sk + 1
                        isnear = j in near

                        def fq(e, j=j, b0=b0, b1=b1, hl=hl, h=h, qg4=qg4, isnear=isnear, vb=vb):
                            for m, bk in ((0, b0), (1, b1)):
                                ins = e.matmul(ps[bk][:, 0:256], KT[vb][64 * m:64 * m + 64, hl, j * 128:(j + 1) * 128],
                                               QTv[64 * m:64 * m + 64, h, qg4 * 256:(qg4 + 1) * 256],
                                               start=True, stop=True, tile_position=(64 * m, 0))
                            if isnear:
                                for m, bk in ((0, b0), (1, b1)):
                                    for ib in range(2):
                                        ty = btype(2 * qg4 + ib, j)
                                        ins = e.matmul(ps[bk][:, ib * 128:(ib + 1) * 128], ident_b[:, :], biasT[:, h, ty, :],
                                                       start=False, stop=True, skip_group_check=True)
                            return ins
                        S.op("pe", fq, reads=[(ktk, hl, j // 4), ("QT", h, qg4 // 2), ("biasT", h), "ident_b"], writes=[PK[b0], PK[b1]])
                        pb = j % 3
                        src2 = psp[sk][:, :].rearrange("p (b n) -> p b n", b=2)[:, :, 0:256]
                        if isnear:
                            S.op("act", lambda e, pb=pb, src2=src2: e.activation(PT[pb], src2, AF.Exp, scale=0.125),
                                 reads=[PK[b0], PK[b1]], writes=["PT%d" % pb])
                        else:
                            fc = far_col(qg4, j)
                            S.op("act", lambda e, pb=pb, src2=src2, fc=fc, h=h: e.activation(
                                PT[pb], src2, AF.Exp, bias=cbB[:, h, fc:fc + 1], scale=0.125),
                                reads=[PK[b0], PK[b1], "cbB"], writes=["PT%d" % pb])
                        if j >= 1:
                            pv(j - 1)
                        if j == 6:
                            for d_ in deferred:
                                d_()
                            deferred.clear()
                        if pend:
                            pend.pop(0)()
                    pv(31)
                    ab = grp % 2
                    grp += 1
                    aS = accS[ab]
                    ask = "accS%d" % ab
                    S.op("dve", lambda e, aS=aS: e.tensor_copy(aS[:, 0:3, :].rearrange("p a b -> p (a b)"), ps[4][:, 0:390]),
                         reads=[PK[4]], writes=[(ask, 0)])
                    S.op("dve", lambda e, aS=aS: e.tensor_copy(aS[:, 3, :], ps[5][:, 0:130]), reads=[PK[5]], writes=[(ask, 1)])
                    rec = stB[:, 0:4]
                    nl = stB[:, 4:6]
                    ss = stB[:, 6:8]
                    rs = stB[:, 8:10]
                    S.op("dve", lambda e, aS=aS: e.reciprocal(stB[:, 0:4].rearrange("p (a b) -> p a b", b=1), aS[:, :, 128:129]),
                         reads=[(ask, 0), (ask, 1)], writes=["rec"])
                    S.op("dve", lambda e: e.tensor_scalar(nl, rec[:, 2:4], nlam, None, ALU.mult), reads=["rec", "nlam"], writes=["nl"])
                    for ib in range(2):
                        S.op("dve", lambda e, ib=ib, aS=aS: e.tensor_scalar(sq_f[:, ib, :], aS[:, 2 + ib, 0:128], nl[:, ib:ib + 1], None, ALU.mult),
                             reads=[(ask, 0), (ask, 1), "nl"], writes=[("sq_f", ib)])
                        S.op("dve", lambda e, ib=ib, aS=aS: e.scalar_tensor_tensor(o_f[:, ib, :], aS[:, ib, 0:128], rec[:, ib:ib + 1], sq_f[:, ib, :], ALU.mult, ALU.add),
                             reads=[(ask, 0), "rec", ("sq_f", ib)], writes=[("o_f", ib)])
                    S.op("dve", lambda e: e.tensor_tensor(sq_f.rearrange("p a b -> p (a b)"), o_f.rearrange("p a b -> p (a b)"), o_f.rearrange("p a b -> p (a b)"), ALU.mult),
                         reads=[("o_f", 0), ("o_f", 1)], writes=[("sq_f", 0), ("sq_f", 1)])
                    S.op("dve", lambda e: e.reduce_sum(ss, sq_f, AX.X), reads=[("sq_f", 0), ("sq_f", 1)], writes=["ss"])
                    S.op("dve", lambda e: e.tensor_scalar(rs, ss, 1.0 / 128, EPS, ALU.mult, ALU.add), reads=["ss"], writes=["rs0"])
                    S.op("pool", lambda e: e.tensor_tensor(rs, rs, mhalf[:, 0:2], ALU.pow), reads=["rs0", "mhalf"], writes=["rs"])
                    abuf = a_bf[ab]
                    abk = "a_bf%d" % ab
                    for ib in range(2):
                        S.op("dve", lambda e, ib=ib, abuf=abuf: e.scalar_tensor_tensor(abuf[:, ib, :], o_f[:, ib, :], rs[:, ib:ib + 1], subg[:, :], ALU.mult, ALU.mult),
                             reads=[("o_f", ib), "rs", "subg"], writes=[(abk, ib)])

                    def fin(abuf=abuf, abk=abk, h=h, qg4=qg4):
                        def ft(e):
                            for ib in range(2):
                                ins = e.transpose(ps6b[:, ib * 128:(ib + 1) * 128], abuf[:, ib, :], ident_b[:, :])
                            return ins
                        S.op("pe", ft, reads=[(abk, 0), (abk, 1), "ident_b"], writes=[PK[6]])
                        S.op("dve", lambda e: e.tensor_copy(asT[:, h, qg4 * 256:(qg4 + 1) * 256], ps6b[:, 0:256]),
                             reads=[PK[6]], writes=[("asT", h, 2 * qg4), ("asT", h, 2 * qg4 + 1)])
                    deferred.append(fin)
            while pend:
                pend.pop(0)()
        for d_ in deferred:
            d_()
        deferred.clear()

        if stage == "B":
            dump("dbg_aT", asT[:, 0:8, :], [128, 8, 1024], BF16, [("asT", h, tt) for h in range(8) for tt in range(8)])
            S.emit(final_wait_keys=outs)
            return nc

        z = AR.alloc("z", 0, F32, [8, 2048])
        wo = [AR.alloc("wo0", 65536, BF16, [16, 512]), AR.alloc("wo1", 81920, BF16, [16, 512])]
        xo = [AR.alloc("xo0", 98304, F32, [512]), AR.alloc("xo1", 100352, F32, [512])]
        lng = AR.alloc("lnCg", 102400, F32, [2048])
        lnb = AR.alloc("lnCb", 110592, F32, [2048])
        h1Tf = AR.alloc("h1Tf", 118784, F32, [16, 128])
        wr = QA.alloc("wr", 0, F32, [16, 36])
        brB = QA.alloc("brB", 2304, F32, [36])
        h1T = asT
        h1Tf2 = [h1Tf, QA.alloc("h1Tf1", 4096, F32, [16, 128])]

        S.dma("sp", wr, wr_d.rearrange("(c p) n -> p c n", p=128), writes=["wr"])
        S.dma("sp", brB, brB_d, writes=["brB"])
        S.dma("sp", lng, ln1g_d, writes=["lnCg"])
        S.dma("sp", lnb, ln1b_d, writes=["lnCb"])
        kxo = 0
        for db in range(4):
            wb = db % 2
            S.dma("pool", wo[wb].rearrange("p a b -> p (a b)"), w_outT[db], writes=["wo%d" % wb])
            for tt in range(8):
                xb = kxo % 2
                kxo += 1
                S.dma("sp", xo[xb], x_own[tt * 128:(tt + 1) * 128, db * 512:(db + 1) * 512], writes=["xo%d" % xb])
                bk = rot4.next()

                def f(e, wb=wb, tt=tt, bk=bk):
                    for c in range(16):
                        ins = e.matmul(ps[bk][:, :], asT[:, c, tt * 128:(tt + 1) * 128], wo[wb][:, c, :],
                                       start=(c == 0), stop=(c == 15))
                    return ins
                S.op("pe", f, reads=["wo%d" % wb] + [("asT", c, tt) for c in range(16)], writes=[PK[bk]])
                S.op("dve", lambda e, xb=xb, tt=tt, db=db, bk=bk: e.scalar_tensor_tensor(
                    z[:, tt, db * 512:(db + 1) * 512], xo[xb], ALPHA, ps[bk][:, :], ALU.mult, ALU.add),
                    reads=["xo%d" % xb, PK[bk]], writes=[("z", tt, db)])
        S.alias("h1T", {"asT"})

        tmpC = []
        for par in range(2):
            base = 126976 + par * 1280
            tmpC.append(dict(
                st=AR.alloc("stC%d" % par, base, F32, [64]),
                Lg=AR.alloc("Lg%d" % par, base + 256, F32, [36]),
                em=AR.alloc("em%d" % par, base + 400, F32, [32]),
                em2=AR.alloc("em2%d" % par, base + 528, F32, [32]),
                oh1=AR.alloc("oh1%d" % par, base + 656, F32, [32]),
                oh2=AR.alloc("oh2%d" % par, base + 784, F32, [32]),
                c1t=AR.alloc("c1t%d" % par, base + 912, F32, [32]),
                rt=AR.alloc("rt%d" % par, base + 1040, F32, [32]),
            ))

        class Chain:
            def __init__(self):
                self.st = []

            def add(self, eng, fn, reads=(), writes=()):
                self.st.append(lambda: S.op(eng, fn, reads=list(reads), writes=list(writes)))

        def ln_stages(ch, tt, stt, lng_, lnb_, gk, bk_, sfx):
            st6 = stt[:, 0:24].rearrange("p (a b) -> p a b", a=4)
            mv = stt[:, 24:26]
            rstd = stt[:, 26:27]
            zk = [("z", tt, db) for db in range(4)]
            for db in range(4):
                ch.add("dve", lambda e, db=db: e.bn_stats(st6[:, db, :], z[:, tt, db * 512:(db + 1) * 512]),
                       reads=[("z", tt, db)], writes=[("st6" + sfx, db)])
            ch.add("dve", lambda e: e.bn_aggr(mv, st6), reads=[("st6" + sfx, db) for db in range(4)], writes=["mv" + sfx])
            ch.add("dve", lambda e: e.tensor_scalar(rstd, mv[:, 1:2], EPS, None, ALU.add), reads=["mv" + sfx], writes=["rstd0" + sfx])
            ch.add("pool", lambda e: e.tensor_tensor(rstd, rstd, mhalf[:, 0:1], ALU.pow), reads=["rstd0" + sfx, "mhalf"], writes=["rstd" + sfx])
            ch.add("dve", lambda e: e.tensor_scalar(z[:, tt, :], z[:, tt, :], mv[:, 0:1], rstd, ALU.subtract, ALU.mult),
                   reads=zk + ["mv" + sfx, "rstd" + sfx], writes=zk)
            ch.add("dve", lambda e: e.tensor_tensor(z[:, tt, :], z[:, tt, :], lng_, ALU.mult), reads=zk + [gk], writes=zk)
            ch.add("dve", lambda e: e.tensor_tensor(z[:, tt, :], z[:, tt, :], lnb_, ALU.add), reads=zk + [bk_], writes=zk)
            return zk

        trot = Rot([4, 5])

        def c_chain(tt):
            par = tt % 2
            tm = tmpC[par]
            sfx = "_c%d" % par
            stt, Lg, em, em2, oh1, oh2, c1t, rt = tm["st"], tm["Lg"], tm["em"], tm["em2"], tm["oh1"], tm["oh2"], tm["c1t"], tm["rt"]
            ch = Chain()
            h1Tf = h1Tf2[par]
            hk = "h1Tf%d" % par
            rb = 6 + par
            zk = ln_stages(ch, tt, stt, lng, lnb, "lnCg", "lnCb", sfx)
            for c4 in range(4):
                def tr(c4=c4):
                    bk = trot.next()

                    def f(e):
                        for k in range(4):
                            c = c4 * 4 + k
                            ins = e.transpose(ps[bk][:, k * 128:(k + 1) * 128], z[:, tt, c * 128:(c + 1) * 128], ident_f[:, :])
                        return ins
                    S.op("pe", f, reads=zk + ["ident_f"], writes=[PK[bk]])
                    src3 = ps[bk][:, :].rearrange("p (a b) -> p a b", a=4)
                    S.op("dve", lambda e: e.tensor_copy(h1Tf[:, c4 * 4:(c4 + 1) * 4, :], src3), reads=[PK[bk]], writes=[(hk, c4)])
                    S.op("act", lambda e: e.activation(h1T[:, c4 * 4:(c4 + 1) * 4, tt * 128:(tt + 1) * 128], h1Tf[:, c4 * 4:(c4 + 1) * 4, :], AF.Copy),
                         reads=[(hk, c4)], writes=[("h1T", c4, tt)])
                ch.st.append(tr)
            ch.add("act", lambda e: e.activation(z[:, tt, :], z[:, tt, :], AF.Copy, scale=ALPHA), reads=zk, writes=zk)

            def fr(e):
                for c in range(16):
                    ins = e.matmul(ps[rb][:, 0:36], h1Tf[:, c, :], wr[:, c, :], start=(c == 0), stop=(c == 15))
                return ins
            ch.add("pe", fr, reads=[(hk, c4) for c4 in range(4)] + ["wr"], writes=[PK[rb]])
            gm, ngm, gs, gg_, v1, v2, dd, ed, w1, w2 = [stt[:, 32 + k:33 + k] for k in range(10)]
            goh, gex, pen = rt[:, 0:4], rt[:, 4:8], rt[:, 8:12]
            K_ = lambda n: n + sfx
            ch.add("dve", lambda e: e.tensor_tensor(Lg, ps[rb][:, 0:36], brB, ALU.add), reads=[PK[rb], "brB"], writes=[K_("Lg")])
            ch.add("dve", lambda e: e.reduce_max(gm, Lg[:, 0:4], AX.X), reads=[K_("Lg")], writes=[K_("gm")])
            ch.add("dve", lambda e: e.tensor_scalar(goh, Lg[:, 0:4], gm, None, ALU.is_equal), reads=[K_("Lg"), K_("gm")], writes=[K_("goh")])
            ch.add("dve", lambda e: e.tensor_scalar(ngm, gm, -1.0, None, ALU.mult), reads=[K_("gm")], writes=[K_("ngm")])
            ch.add("act", lambda e: e.activation(gex, Lg[:, 0:4], AF.Exp, bias=ngm), reads=[K_("Lg"), K_("ngm")], writes=[K_("gex")])
            ch.add("dve", lambda e: e.reduce_sum(gs, gex, AX.X), reads=[K_("gex")], writes=[K_("gs")])
            ch.add("dve", lambda e: e.reciprocal(gg_, gs), reads=[K_("gs")], writes=[K_("gg")])
            ch.add("dve", lambda e: e.tensor_scalar(pen, goh, -1.0, 1e30, ALU.add, ALU.mult), reads=[K_("goh")], writes=[K_("pen")])
            for g in range(4):
                ch.add("dve", lambda e, g=g: e.tensor_scalar(em[:, g * 8:(g + 1) * 8], Lg[:, 4 + g * 8:12 + g * 8], pen[:, g:g + 1], None, ALU.add),
                       reads=[K_("Lg"), K_("pen")], writes=[(K_("em"), g)])
            emk = [(K_("em"), g) for g in range(4)]
            ch.add("dve", lambda e: e.reduce_max(v1, em, AX.X), reads=emk, writes=[K_("v1")])
            ch.add("dve", lambda e: e.tensor_scalar(oh1, em, v1, None, ALU.is_equal), reads=emk + [K_("v1")], writes=[K_("oh1")])
            ch.add("dve", lambda e: e.scalar_tensor_tensor(em2, oh1, -1e30, em, ALU.mult, ALU.add), reads=emk + [K_("oh1")], writes=[K_("em2")])
            ch.add("dve", lambda e: e.reduce_max(v2, em2, AX.X), reads=[K_("em2")], writes=[K_("v2")])
            ch.add("dve", lambda e: e.tensor_scalar(oh2, em2, v2, None, ALU.is_equal), reads=[K_("em2"), K_("v2")], writes=[K_("oh2")])
            ch.add("dve", lambda e: e.tensor_tensor(dd, v2, v1, ALU.subtract), reads=[K_("v1"), K_("v2")], writes=[K_("dd")])
            ch.add("act", lambda e: e.activation(ed, dd, AF.Exp), reads=[K_("dd")], writes=[K_("ed")])
            ch.add("dve", lambda e: e.tensor_scalar(w1, ed, 1.0, None, ALU.add), reads=[K_("ed")], writes=[K_("den")])
            ch.add("dve", lambda e: e.reciprocal(w1, w1), reads=[K_("den")], writes=[K_("w1a")])
            ch.add("dve", lambda e: e.tensor_tensor(w1, w1, gg_, ALU.mult), reads=[K_("w1a"), K_("gg")], writes=[K_("w1")])
            ch.add("dve", lambda e: e.tensor_tensor(w2, w1, ed, ALU.mult), reads=[K_("w1"), K_("ed")], writes=[K_("w2")])
            ch.add("dve", lambda e: e.tensor_scalar(c1t, oh1, w1, None, ALU.mult), reads=[K_("oh1"), K_("w1")], writes=[K_("c1t")])
            ch.add("dve", lambda e: e.scalar_tensor_tensor(comb[:, tt, :], oh2, w2, c1t, ALU.mult, ALU.add),
                   reads=[K_("oh2"), K_("w2"), K_("c1t")], writes=[("comb", tt)])
            ch.add("dve", lambda e: e.tensor_copy(OH[:, tt, 0:32], oh1), reads=[K_("oh1")], writes=[("OH1", tt)])
            ch.add("dve", lambda e: e.tensor_copy(OH[:, tt, 32:64], oh2), reads=[K_("oh2")], writes=[("OH2", tt)])
            ch.add("dve", lambda e: e.tensor_copy(W12[:, tt, 0:1], w1), reads=[K_("w1")], writes=[("W1", tt)])
            ch.add("dve", lambda e: e.tensor_copy(W12[:, tt, 1:2], w2), reads=[K_("w2")], writes=[("W2", tt)])
            return ch.st

        interleave([c_chain(tt) for tt in range(8)], 2)

        if stage == "C":
            o = dout("dbg_h1a", [NT, D], F32)
            for tt in range(8):
                S.dma("sp", o[tt * 128:(tt + 1) * 128, :], z[:, tt, :], reads=[("z", tt, db) for db in range(4)], writes=[("dbg_h1a", tt)])
                outs.append(("dbg_h1a", tt))
            dump("dbg_comb", comb[:, :, :], [128, 8, 32], F32, [("comb", tt) for tt in range(8)])
            dump("dbg_h1T", h1T[:, :, :], [128, 16, 1024], BF16, [("h1T", c4, tt) for c4 in range(4) for tt in range(8)])
            S.emit(final_wait_keys=outs)
            return nc

        if MOE_SPARSE:
            I32 = mybir.dt.int32
            wgu = [AR.alloc("wgu0", 65536, BF16, [16, 512]), AR.alloc("wgu1", 90112, BF16, [16, 512])]
            wdn = [AR.alloc("wdn0", 81920, BF16, [2, 2048]), AR.alloc("wdn1", 106496, BF16, [2, 2048])]
            lng2 = AR.alloc("lnDg", 114688, F32, [2048])
            lnb2 = AR.alloc("lnDb", 122880, F32, [2048])
            HA = Arena(S, asT[:, :, :].rearrange("p a b -> p (a b)"))
            HA.live.append((0, 32768, "asT"))
            HA.live.append((0, 32768, "h1T"))
            xs = [HA.alloc("xs0", 0, BF16, [2048]), HA.alloc("xs1", 4096, BF16, [2048])]
            xsT = HA.alloc("xsT", 8192, BF16, [16, 128])
            ys = HA.alloc("ys", 12288, BF16, [2048])
            h1b = HA.alloc("h1b", 16384, BF16, [2048])
            yg = [HA.alloc("yg0", 20480, BF16, [2048]), HA.alloc("yg1", 24576, BF16, [2048])]
            sgt1 = HA.alloc("sgt1", 28672, F32, [256])
            actb1 = HA.alloc("actb1", 29696, BF16, [256])
            actT1 = HA.alloc("actT1", 30208, BF16, [2, 128])
            stD = QA.alloc("stD", 12288, F32, [64])
            A_b = QA.alloc("A_b", 4096, BF16, [8, 32])
            R_sb = QA.alloc("R_sb", 4608, F32, [8, 32])
            n_sb = QA.alloc("n_sb", 5632, F32, [32])
            nt_sb = QA.alloc("nt_sb", 5760, F32, [32])
            cs = [QA.alloc("cs0", 5888, F32, [32]), QA.alloc("cs1", 6016, F32, [32])]
            tb_sb = QA.alloc("tb_sb", 6144, F32, [32])
            G_sb = QA.alloc("G_sb", 6272, F32, [32])
            tmp32 = QA.alloc("tmp32", 6400, F32, [32])
            ej_f = QA.alloc("ej_f", 6528, F32, [NSL])
            ej_i = QA.alloc("ej_i", 6720, I32, [NSL])
            sl_f = QA.alloc("sl_f", 6912, F32, [8, 2])
            sl_i = QA.alloc("sl_i", 6976, I32, [8, 2])
            tri_b = QA.alloc("tri_b", 7040, BF16, [128])
            one_b = QA.alloc("one_b", 7296, BF16, [128])

            S.dma("pool", tri_b, tri_d, writes=["tri_b"])
            iota_p = QA.alloc("iota_p", 7552, F32, [1])
            S.dma("sp", iota_p, iota_d, writes=["iota_p"])
            S.op("dve", lambda e: e.memset(one_b, 1.0), writes=["one_b"])
            ohk = [("OH1", tt) for tt in range(8)] + [("OH2", tt) for tt in range(8)]
            S.op("dve", lambda e: e.tensor_tensor(A_b, OH[:, :, 0:32], OH[:, :, 32:64], ALU.add), reads=ohk, writes=["A_b"])
            for tt in range(9):
                bk = tt % 4

                def f(e, tt=tt, bk=bk):
                    if tt < 8:
                        mm = [(one_b, t2) for t2 in range(tt)] + [(tri_b, tt)]
                    else:
                        mm = [(one_b, t2) for t2 in range(8)]
                    for n_, (lh, t2) in enumerate(mm):
                        ins = e.matmul(ps[bk][:, 0:32], lh, A_b[:, t2, :], start=(n_ == 0), stop=(n_ == len(mm) - 1))
                    return ins
                S.op("pe", f, reads=["A_b", "tri_b", "one_b"], writes=[PK[bk]])
                if tt < 8:
                    S.op("dve", lambda e, tt=tt, bk=bk: e.tensor_copy(R_sb[:, tt, :], ps[bk][:, 0:32]), reads=[PK[bk]], writes=[("R_sb", tt)])
                else:
                    S.op("dve", lambda e, bk=bk: e.tensor_copy(n_sb, ps[bk][:, 0:32]), reads=[PK[bk]], writes=["n_sb"])
            S.op("dve", lambda e: e.tensor_scalar(nt_sb, n_sb, 0.0, None, ALU.is_gt), reads=["n_sb"], writes=["nt_sb"])
            for k in range(1, 8):
                S.op("dve", lambda e, k=k: e.tensor_scalar(tmp32, n_sb, 128.0 * k, None, ALU.is_gt), reads=["n_sb"], writes=["tmp32"])
                S.op("dve", lambda e: e.tensor_tensor(nt_sb, nt_sb, tmp32, ALU.add), reads=["nt_sb", "tmp32"], writes=["nt_sb"])
            S.op("dve", lambda e: e.tensor_copy(cs[0], nt_sb), reads=["nt_sb"], writes=["cs0"])
            cur = 0
            for sh in (1, 2, 4, 8, 16):
                a_, b_ = cs[cur], cs[1 - cur]
                ka, kb = "cs%d" % cur, "cs%d" % (1 - cur)
                S.op("dve", lambda e, a_=a_, b_=b_, sh=sh: e.tensor_copy(b_[:, 0:sh], a_[:, 0:sh]), reads=[ka], writes=[(kb, 0)])
                S.op("dve", lambda e, a_=a_, b_=b_, sh=sh: e.tensor_tensor(b_[:, sh:32], a_[:, sh:32], a_[:, 0:32 - sh], ALU.add),
                     reads=[ka, (ka, 0), (ka, 1)], writes=[(kb, 1), kb])
                cur = 1 - cur
            te_sb = cs[cur]
            tek = "cs%d" % cur
            S.op("dve", lambda e: e.tensor_tensor(tb_sb, te_sb, nt_sb, ALU.subtract), reads=[tek, "nt_sb"], writes=["tb_sb"])
            for j in range(NSL):
                S.op("dve", lambda e, j=j: e.tensor_scalar(tmp32, te_sb, float(j), None, ALU.is_le), reads=[tek], writes=["tmp32"])
                S.op("dve", lambda e, j=j: e.reduce_sum(ej_f[:, j:j + 1], tmp32, AX.X), reads=["tmp32"], writes=[("ej_f", j)])
            S.op("dve", lambda e: e.tensor_scalar(ej_f, ej_f, 128.0, iota_p, ALU.mult, ALU.add), reads=[("ej_f", j) for j in range(NSL)] + ["iota_p"], writes=["ej_f2"])
            S.op("dve", lambda e: e.tensor_copy(ej_i, ej_f), reads=["ej_f2"], writes=["ej_i"])
            for tt in range(8):
                S.op("dve", lambda e, tt=tt: e.scalar_tensor_tensor(G_sb, tb_sb, 128.0, R_sb[:, tt, :], ALU.mult, ALU.add),
                     reads=["tb_sb", ("R_sb", tt)], writes=["G_sb"])
                for c_ in range(2):
                    S.op("dve", lambda e, tt=tt, c_=c_: e.tensor_tensor(tmp32, G_sb, OH[:, tt, 32 * c_:32 * c_ + 32], ALU.mult),
                         reads=["G_sb"] + ohk, writes=["tmp32"])
                    S.op("dve", lambda e, tt=tt, c_=c_: e.reduce_sum(sl_f[:, tt, c_:c_ + 1], tmp32, AX.X), reads=["tmp32"], writes=[("sl_f", tt, c_)])
            S.op("dve", lambda e: e.tensor_copy(sl_i.rearrange("p a b -> p (a b)"), sl_f.rearrange("p a b -> p (a b)")),
                 reads=[("sl_f", tt, c_) for tt in range(8) for c_ in range(2)], writes=["sl_i"])
            if stage == "M":
                dump("dbg_slf", sl_f, [128, 8, 2], F32, [("sl_f", tt, c_) for tt in range(8) for c_ in range(2)])
                dump("dbg_sli", sl_i, [128, 8, 2], I32, ["sl_i"])
                dump("dbg_ejf", ej_f, [128, NSL], F32, ["ej_f2"])
                dump("dbg_eji", ej_i, [128, NSL], I32, ["ej_i"])
                dump("dbg_n", n_sb, [128, 32], F32, ["n_sb"])
                dump("dbg_R", R_sb, [128, 8, 32], F32, [("R_sb", tt) for tt in range(8)])
                dump("dbg_OH", OH[:, :, :], [128, 8, 64], F32, ohk)
                dump("dbg_tb", tb_sb, [128, 32], F32, ["tb_sb"])
                S.emit(final_wait_keys=outs)
                return nc
            for tt in range(8):
                zk = [("z", tt, db) for db in range(4)]
                S.op("act", lambda e, tt=tt: e.activation(h1b, z[:, tt, :], AF.Copy, scale=1.0 / ALPHA), reads=zk, writes=["h1b"])
                for c_ in range(2):
                    def fsc(eng, tt=tt, c_=c_):
                        return eng.indirect_dma_start(out=Xslots[:, :], out_offset=bass.IndirectOffsetOnAxis(ap=sl_i[:, tt, c_:c_ + 1], axis=0),
                                                      in_=h1b, in_offset=None)
                    S.dma_fn("pool", fsc, reads=["h1b", "sl_i", "Xzero"], writes=[("Xslots", tt, c_)])
            xsk = [("Xslots", tt, c_) for tt in range(8) for c_ in range(2)]
            if stage == "S1":
                S.dma("sp", xs[0], Xslots[0:128, :], reads=xsk, writes=["xs0"])
                dump("dbg_xs", xs[0], [128, 2048], BF16, ["xs0"])
                dump("dbg_sli", sl_i, [128, 8, 2], I32, ["sl_i"])
                S.emit(final_wait_keys=outs)
                return nc
            lng2_loaded = False
            bndbox = {}
            ps0b = ps[0][:, :].bitcast(BF16)
            ps1b = ps[1][:, :].bitcast(BF16)
            ps3b = ps[3][:, :].bitcast(BF16)
            for j in range(NSL):
                b = j % 2

                def fw1(eng, j=j, b=b):
                    if "r" not in bndbox:
                        r_ = eng.alloc_register("moe_bnd")
                        eng.reg_mov(r_, 32 * 128 - 1)
                        bndbox["r"] = r_
                    return eng.indirect_dma_start(out=wgu[b].rearrange("p a b -> p (a b)"), out_offset=None,
                                                  in_=w_gu.rearrange("e p n -> (e p) n"),
                                                  in_offset=bass.IndirectOffsetOnAxis(ap=ej_i[:, j:j + 1], axis=0),
                                                  bounds_check=bndbox["r"], oob_is_err=False)

                def fw2(eng, j=j, b=b):
                    return eng.indirect_dma_start(out=wdn[b].rearrange("p a b -> p (a b)"), out_offset=None,
                                                  in_=w_dn.rearrange("e p n -> (e p) n"),
                                                  in_offset=bass.IndirectOffsetOnAxis(ap=ej_i[:, j:j + 1], axis=0),
                                                  bounds_check=bndbox["r"], oob_is_err=False)
                S.dma_fn("pool", fw1, reads=["ej_i"], writes=[("wgu%d" % b, 0), ("wgu%d" % b, 1)])
                S.dma_fn("pool", fw2, reads=["ej_i"], writes=["wdn%d" % b])
                if stage == "S2" and j == 1:
                    dump("dbg_wgu", wgu[0][:, 0:2, :], [128, 2, 512], BF16, [("wgu0", 0), ("wgu0", 1)])
                    dump("dbg_wdn", wdn[0][:, 0, 0:512], [128, 512], BF16, ["wdn0"])
                    S.emit(final_wait_keys=outs)
                    return nc
                xb = xs[b]
                S.dma("sp", xb, Xslots[j * 128:(j + 1) * 128, :], reads=xsk, writes=["xs%d" % b])

                def ft1(e, xb=xb):
                    for c in range(8):
                        ins = e.transpose(ps0b[:, c * 128:(c + 1) * 128], xb[:, c * 128:(c + 1) * 128], ident_b[:, :])
                    return ins

                def ft2(e, xb=xb):
                    for c in range(8, 16):
                        ins = e.transpose(ps1b[:, (c - 8) * 128:(c - 7) * 128], xb[:, c * 128:(c + 1) * 128], ident_b[:, :])
                    return ins
                S.op("pe", ft1, reads=["xs%d" % b, "ident_b"], writes=[PK[0]])
                S.op("pe", ft2, reads=["xs%d" % b, "ident_b"], writes=[PK[1]])
                S.op("dve", lambda e: e.tensor_copy(xsT[:, 0:8, :].rearrange("p a b -> p (a b)"), ps0b[:, 0:1024]), reads=[PK[0]], writes=[("xsT", 0)])
                S.op("act", lambda e: e.activation(xsT[:, 8:16, :].rearrange("p a b -> p (a b)"), ps1b[:, 0:1024], AF.Copy), reads=[PK[1]], writes=[("xsT", 1)])

                def fg(e, b=b):
                    for c in range(16):
                        ins = e.matmul(ps[2][:, :], xsT[:, c, :], wgu[b][:, c, :], start=(c == 0), stop=(c == 15))
                    return ins
                S.op("pe", fg, reads=[("xsT", 0), ("xsT", 1), ("wgu%d" % b, 0), ("wgu%d" % b, 1)], writes=[PK[2]])
                S.op("act", lambda e: e.activation(sgt1, ps[2][:, 0:256], AF.Silu), reads=[PK[2]], writes=["sgt1"])
                S.op("dve", lambda e: e.tensor_tensor(actb1, sgt1, ps[2][:, 256:512], ALU.mult), reads=["sgt1", PK[2]], writes=["actb1"])

                def fta(e):
                    for fc in range(2):
                        ins = e.transpose(ps3b[:, fc * 128:(fc + 1) * 128], actb1[:, fc * 128:(fc + 1) * 128], ident_b[:, :])
                    return ins
                S.op("pe", fta, reads=["actb1", "ident_b"], writes=[PK[3]])
                S.op("act", lambda e: e.activation(actT1.rearrange("p a b -> p (a b)"), ps3b[:, 0:256], AF.Copy), reads=[PK[3]], writes=["actT1"])

                def fd(e, b=b):
                    for db in range(4):
                        for fc in range(2):
                            ins = e.matmul(ps[4 + db][:, :], actT1[:, fc, :], wdn[b][:, fc, db * 512:(db + 1) * 512],
                                           start=(fc == 0), stop=(fc == 1))
                    return ins
                S.op("pe", fd, reads=["actT1", "wdn%d" % b], writes=[PK[4], PK[5], PK[6], PK[7]])
                for db in range(4):
                    if db % 2 == 0:
                        S.op("dve", lambda e, db=db: e.tensor_copy(ys[:, db * 512:(db + 1) * 512], ps[4 + db][:, :]), reads=[PK[4 + db]], writes=[("ys", db)])
                    else:
                        S.op("act", lambda e, db=db: e.activation(ys[:, db * 512:(db + 1) * 512], ps[4 + db][:, :], AF.Copy), reads=[PK[4 + db]], writes=[("ys", db)])
                S.dma("sp", Yslots[j * 128:(j + 1) * 128, :], ys, reads=[("ys", db) for db in range(4)], writes=[("Yslots", j)])
                if stage.startswith("T") and j + 1 == int(stage[1:]):
                    dump("dbg_ys", ys, [128, 2048], BF16, [("ys", db) for db in range(4)])
                    dump("dbg_xsT", xsT, [128, 16, 128], BF16, [("xsT", 0), ("xsT", 1)])
                    outs.append(("Yslots", j))
                    S.emit(final_wait_keys=outs)
                    return nc
            ysk = [("Yslots", j) for j in range(NSL)]
            for tt in range(8):
                for c_ in range(2):
                    def fga(eng, tt=tt, c_=c_):
                        return eng.indirect_dma_start(out=yg[c_], out_offset=None, in_=Yslots[:, :],
                                                      in_offset=bass.IndirectOffsetOnAxis(ap=sl_i[:, tt, c_:c_ + 1], axis=0))
                    S.dma_fn("pool", fga, reads=ysk + ["sl_i"], writes=["yg%d" % c_])
                    S.op("dve", lambda e, tt=tt, c_=c_: e.scalar_tensor_tensor(z[:, tt, :], yg[c_], W12[:, tt, c_:c_ + 1], z[:, tt, :], ALU.mult, ALU.add),
                         reads=["yg%d" % c_, ("W1", tt), ("W2", tt)] + [("z", tt, db) for db in range(4)], writes=[("z", tt, db) for db in range(4)])

        else:
            wgu = [AR.alloc("wgu0", 65536, BF16, [16, 512]), AR.alloc("wgu1", 90112, BF16, [16, 512])]
            wdn = [AR.alloc("wdn0", 81920, BF16, [2, 2048]), AR.alloc("wdn1", 106496, BF16, [2, 2048])]
            lng2 = AR.alloc("lnDg", 114688, F32, [2048])
            lnb2 = AR.alloc("lnDb", 122880, F32, [2048])
            sgt = [QA.alloc("sgt%d" % k, 4096 + k * 1024, F32, [256]) for k in range(3)]
            actb = [QA.alloc("actb%d" % k, 8192 + k * 512, BF16, [256]) for k in range(3)]
            actT = [QA.alloc("actT%d" % k, 10240 + k * 512, BF16, [2, 128]) for k in range(3)]
            stD = QA.alloc("stD", 12288, F32, [64])

            N = 32 * 8
            ps2b = ps[2][:, :].bitcast(BF16)

            def load_expert(ex):
                b = ex % 2
                S.dma("pool", wgu[b].rearrange("p a b -> p (a b)"), w_gu[ex], writes=[("wgu%d" % b, 0), ("wgu%d" % b, 1)])
                S.dma("pool", wdn[b].rearrange("p a b -> p (a b)"), w_dn[ex], writes=["wdn%d" % b])

            load_expert(0)
            for step in range(N + 2):
                if step < N:
                    k = step
                    ex, tt = divmod(k, 8)
                    b = ex % 2
                    gb = k % 2
                    r3 = k % 3

                    def f(e, b=b, tt=tt, gb=gb):
                        for c in range(16):
                            ins = e.matmul(ps[gb][:, :], h1T[:, c, tt * 128:(tt + 1) * 128], wgu[b][:, c, :],
                                           start=(c == 0), stop=(c == 15))
                        return ins
                    S.op("pe", f, reads=[("wgu%d" % b, 0), ("wgu%d" % b, 1)] + [("h1T", c4, tt) for c4 in range(4)], writes=[PK[gb]])
                    S.op("act", lambda e, r3=r3, gb=gb: e.activation(sgt[r3], ps[gb][:, 0:256], AF.Silu),
                         reads=[PK[gb]], writes=["sgt%d" % r3])
                    S.op("dve", lambda e, r3=r3, gb=gb, tt=tt, ex=ex: e.scalar_tensor_tensor(
                        actb[r3], sgt[r3], comb[:, tt, ex:ex + 1], ps[gb][:, 256:512], ALU.mult, ALU.mult),
                        reads=["sgt%d" % r3, PK[gb], ("comb", tt)], writes=["actb%d" % r3])
                if 1 <= step <= N:
                    k = step - 1
                    r3 = k % 3

                    def ft(e, r3=r3):
                        for fc in range(2):
                            ins = e.transpose(ps2b[:, fc * 128:(fc + 1) * 128], actb[r3][:, fc * 128:(fc + 1) * 128], ident_b[:, :])
                        return ins
                    S.op("pe", ft, reads=["actb%d" % r3, "ident_b"], writes=[PK[2]])
                    S.op("act", lambda e, r3=r3: e.activation(actT[r3].rearrange("p a b -> p (a b)"), ps2b[:, 0:256], AF.Copy),
                         reads=[PK[2]], writes=["actT%d" % r3])
                if step >= 2:
                    k = step - 2
                    ex, tt = divmod(k, 8)
                    b = ex % 2
                    r3 = k % 3

                    def fd(e, b=b, r3=r3):
                        for db in range(4):
                            for fc in range(2):
                                ins = e.matmul(ps[4 + db][:, :], actT[r3][:, fc, :], wdn[b][:, fc, db * 512:(db + 1) * 512],
                                               start=(fc == 0), stop=(fc == 1))
                        return ins
                    S.op("pe", fd, reads=["actT%d" % r3, "wdn%d" % b], writes=[PK[4], PK[5], PK[6], PK[7]])
                    for db in range(4):
                        S.op("dve", lambda e, tt=tt, db=db: e.tensor_tensor(
                            z[:, tt, db * 512:(db + 1) * 512], z[:, tt, db * 512:(db + 1) * 512], ps[4 + db][:, :], ALU.add),
                            reads=[PK[4 + db], ("z", tt, db)], writes=[("z", tt, db)])
                if step % 8 == 1 and step // 8 + 1 < 32:
                    load_expert(step // 8 + 1)


        S.dma("sp", lng2, ln2g_d, writes=["lnDg"])
        S.dma("sp", lnb2, ln2b_d, writes=["lnDb"])
        out = dout("out", [NT, D], F32)

        stD2 = [stD, QA.alloc("stD1", 12544, F32, [64])]

        def d_chain(tt):
            ch = Chain()
            zk = ln_stages(ch, tt, stD2[tt % 2], lng2, lnb2, "lnDg", "lnDb", "_d%d" % (tt % 2))

            def fin():
                S.dma("sp", out[tt * 128:(tt + 1) * 128, :], z[:, tt, :], reads=zk, writes=[("out", tt)])
                outs.append(("out", tt))
            ch.st.append(fin)
            return ch.st

        interleave([d_chain(tt) for tt in range(8)], 2)
        S.emit(final_wait_keys=outs)
    return nc


def _bucket_table():
    rel = np.arange(-(SEQ - 1), SEQ, dtype=np.int32)
    try:
        import jax
        import jax.numpy as jnp
        cpu = jax.devices("cpu")[0]
        with jax.default_device(cpu):
            r = jnp.asarray(rel)
            half, max_exact = 16, 8
            ret = jnp.where(r > 0, half, 0)
            n = jnp.abs(r)
            nf = jnp.maximum(n, 1).astype(jnp.float32)
            large = max_exact + (jnp.log(nf / max_exact) / math.log(128 / max_exact) * (half - max_exact)).astype(jnp.int32)
            large = jnp.minimum(large, half - 1)
            b = np.asarray(ret + jnp.where(n < max_exact, n, large))
    except Exception:
        half, max_exact = 16, 8
        ret = np.where(rel > 0, half, 0)
        n = np.abs(rel)
        nf = np.maximum(n, 1).astype(np.float32)
        large = max_exact + (np.log(nf / np.float32(max_exact)) / np.float32(math.log(128 / max_exact)) * np.float32(half - max_exact)).astype(np.int32)
        large = np.minimum(large, half - 1)
        b = ret + np.where(n < max_exact, n, large)
    return {int(r): int(v) for r, v in zip(rel, b)}


def _etables(r, bt):
    E1 = np.zeros((32, NTYPES, 256), np.float32)
    E2 = np.zeros((32, 26), np.float32)

    def toep(ty, Dblk):
        for i in range(255):
            E1[bt[128 * Dblk + 127 - i], ty, i] = 1.0

    def const(ty, b):
        E1[b, ty, :255] = 1.0
    toep(0, 0)
    toep(1, 1)
    toep(2, -1)
    const(3, 31)
    const(4, 15)
    if r < 3:
        toep(5, 1)
        const(6, 31)
    else:
        const(5, 15)
        const(6, 15)
    if r > 0:
        toep(7, -1)
        const(8, 15)
    else:
        const(7, 31)
        const(8, 31)
    for j in range(8, 32):
        wrapped = (8 * r + j) >= 32
        E2[15 if wrapped else 31, j - 8] = 1.0
    E2[15, 24] = 1.0
    E2[31, 25] = 1.0
    return E1.reshape(32, NTYPES * 256), E2


_NC_CACHE = {}


def make_in_maps(inputs):
    f = lambda a: np.ascontiguousarray(np.asarray(a, dtype=np.float32))
    x = f(inputs["x"])
    bt = _bucket_table()
    rep = lambda v, n=128: np.ascontiguousarray(np.broadcast_to(np.asarray(v, np.float32).reshape(1, -1), (n, np.asarray(v).size)))

    def ptile(w):
        K, n = w.shape
        return np.ascontiguousarray(w.reshape(K // 128, 128, n).transpose(1, 0, 2).reshape(128, (K // 128) * n))

    w_in = f(inputs["w_in"][0])
    w_out = f(inputs["w_out"][0])
    weg = f(inputs["w_exp_gate"][0])
    weu = f(inputs["w_exp_up"][0])
    wed = f(inputs["w_exp_down"][0])
    w_inA = np.stack([ptile(w_in[:, c0:c0 + 512]) for c0 in (0, 512, 3072, 3584, 4096, 4608)])
    w_kv = np.stack([np.stack([ptile(w_in[:, 1024 + hg * 256:1024 + (hg + 1) * 256]),
                               ptile(w_in[:, 2048 + hg * 256:2048 + (hg + 1) * 256])]) for hg in range(4)])
    w_outT = np.stack([ptile(w_out[:, db * 512:(db + 1) * 512]) for db in range(4)])
    w_gu = np.stack([ptile(np.concatenate([weg[e], weu[e]], axis=1)) for e in range(32)])
    w_dn = np.stack([ptile(wed[e]) for e in range(32)])
    wr_cat = np.ascontiguousarray(np.concatenate(
        [f(inputs["w_router_group"][0])] + [f(inputs["w_router_expert"][0][g]) for g in range(4)], axis=1))
    br = np.concatenate([f(inputs["b_router_group"][0]).reshape(-1), f(inputs["b_router_expert"][0]).reshape(-1)])
    lamp = np.concatenate([f(inputs[k][0]).reshape(-1) for k in ("lam_q1", "lam_k1", "lam_q2", "lam_k2")])
    common = {
        "w_inA": w_inA, "w_kv": w_kv, "w_outT": w_outT, "w_gu": w_gu, "w_dn": w_dn,
        "wr_cat": wr_cat, "br_b": rep(br),
        "ln1g_b": rep(inputs["ln1_g"][0]), "ln1b_b": rep(inputs["ln1_b"][0]),
        "ln2g_b": rep(inputs["ln2_g"][0]), "ln2b_b": rep(inputs["ln2_b"][0]),
        "rel_bias": f(inputs["rel_bias"]), "lamp_b": rep(lamp), "subg_b": rep(inputs["subln_g"][0]),
        "sglng_b": rep(f(inputs["sg_ln_g"][0]).reshape(-1)), "sglnb_b": rep(f(inputs["sg_ln_b"][0]).reshape(-1)),
        "sg_wT": np.ascontiguousarray(f(inputs["sg_w"][0]).transpose(0, 2, 1)),
        "sg_b_row": f(inputs["sg_b"][0]).reshape(1, 1024),
        "ident": np.eye(128, dtype=np.float32),
    }
    if MOE_SPARSE:
        common["tri"] = np.ascontiguousarray(np.triu(np.ones((128, 128), np.float32), 1))
        common["iota_p"] = np.arange(128, dtype=np.float32).reshape(128, 1)
    in_maps = []
    for c in range(8):
        b, r = divmod(c, 4)
        idx = np.concatenate([((8 * r + j) % 32) * 128 + (127 - np.arange(128)) for j in range(32)])
        E1, E2 = _etables(r, bt)
        m = dict(common)
        xTa = x[b][idx].T
        m["xT_all"] = np.ascontiguousarray(xTa.reshape(16, 128, 16, 256).transpose(2, 1, 0, 3).reshape(16, 128, 4096))
        m["xT_own"] = ptile(np.ascontiguousarray(x[b, r * NT:(r + 1) * NT].T))
        m["x_own"] = np.ascontiguousarray(x[b, r * NT:(r + 1) * NT])
        m["E1"] = E1
        m["E2"] = E2
        in_maps.append(m)
    return in_maps


def kernel(**inputs):
    if "nc" not in _NC_CACHE:
        _NC_CACHE["nc"] = build("D")
    nc = _NC_CACHE["nc"]
    in_maps = make_in_maps(inputs)
    res = run_bass_kernel_spmd(nc, in_maps, core_ids=list(range(8)))
    out = np.zeros((2, SEQ, D), np.float32)
    for c in range(8):
        b, r = divmod(c, 4)
        out[b, r * NT:(r + 1) * NT] = res.results[c]["out"]
    return out
```

```python
import contextlib
import math
import os
import numpy as np
import concourse.bass as bass
import concourse.mybir as mybir
from concourse.bass_utils import run_bass_kernel_spmd

F32 = mybir.dt.float32
BF16 = mybir.dt.bfloat16
AF = mybir.ActivationFunctionType
ALU = mybir.AluOpType
AX = mybir.AxisListType

D = 2048
SEQ = 4096
NT = 1024
ALPHA = 2.0 ** 0.25
EPS = 1e-5
LAM_INIT = 0.8 - 0.6 * math.exp(0.0)
NTYPES = 9
MOE_SPARSE = True


def _name_of(k):
    return k[0] if isinstance(k, tuple) else k


class Sched:
    ENGS = ("pe", "act", "dve", "pool", "sp")

    def __init__(self, nc, es, n_dma_sems=48, same_engine_waits=("act", "dve", "pool")):
        self.nc = nc
        self.ops = {e: [] for e in self.ENGS}
        self.count = {e: 0 for e in self.ENGS}
        self.sem = {e: es.enter_context(nc.semaphore("prog_" + e)) for e in self.ENGS}
        self.dsems = [es.enter_context(nc.semaphore("dma%d" % i)) for i in range(n_dma_sems)]
        self.dval = [0] * n_dma_sems
        self.dnext = 0
        self.waited = {e: {} for e in self.ENGS}
        self.last_w = {}
        self.readers = {}
        self.pending = {}
        self.same = set(same_engine_waits)

    def alias(self, newname, oldnames):
        toks = {}
        for k, t in self.last_w.items():
            if t is not None and _name_of(k) in oldnames:
                toks[t[0]] = max(toks.get(t[0], 0), t[1])
        for k, ts in self.readers.items():
            if _name_of(k) in oldnames:
                for t in ts:
                    toks[t[0]] = max(toks.get(t[0], 0), t[1])
        self.pending[newname] = list(toks.items())

    def _init_key(self, k):
        if k not in self.last_w and k not in self.readers:
            self.last_w[k] = None
            self.readers[k] = list(self.pending.get(_name_of(k), []))

    def _deps(self, eng, reads, writes):
        toks = []
        for k in reads:
            self._init_key(k)
            t = self.last_w.get(k)
            if t is not None:
                toks.append(t)
        for k in writes:
            self._init_key(k)
            t = self.last_w.get(k)
            if t is not None:
                toks.append(t)
            toks.extend(self.readers.get(k, ()))
        need = {}
        for (s, v) in toks:
            if isinstance(s, str) and s == eng and eng not in self.same:
                continue
            if v > need.get(s, 0):
                need[s] = v
        out = []
        for s, v in need.items():
            if self.waited[eng].get(s, 0) >= v:
                continue
            self.waited[eng][s] = v
            out.append((s, v))
        return out

    def _record(self, tok, reads, writes):
        for k in writes:
            self.last_w[k] = tok
            self.readers[k] = []
        for k in reads:
            if k in writes:
                continue
            self.readers.setdefault(k, []).append(tok)

    def op(self, eng, fn, reads=(), writes=()):
        waits = self._deps(eng, reads, writes)
        self.count[eng] += 1
        tok = (eng, self.count[eng])
        self.ops[eng].append(("op", fn, waits))
        self._record(tok, reads, writes)
        return tok

    def dma(self, queue, out, in_, reads=(), writes=(), **kw):
        i = self.dnext
        self.dnext = (self.dnext + 1) % len(self.dsems)
        waits = self._deps(queue, reads, writes)
        prev = self.dval[i]
        if prev > 0 and self.waited[queue].get(i, 0) < prev:
            self.waited[queue][i] = prev
            waits.append((i, prev))
        self.dval[i] += 16
        tok = (i, self.dval[i])
        self.ops[queue].append(("dma", (out, in_, kw), waits, i))
        self._record(tok, reads, writes)
        return tok

    def dma_fn(self, queue, fn, reads=(), writes=()):
        i = self.dnext
        self.dnext = (self.dnext + 1) % len(self.dsems)
        waits = self._deps(queue, reads, writes)
        prev = self.dval[i]
        if prev > 0 and self.waited[queue].get(i, 0) < prev:
            self.waited[queue][i] = prev
            waits.append((i, prev))
        self.dval[i] += 16
        tok = (i, self.dval[i])
        self.ops[queue].append(("dmaf", fn, waits, i))
        self._record(tok, reads, writes)
        return tok

    def _semof(self, s):
        return self.sem[s] if isinstance(s, str) else self.dsems[s]

    def emit(self, final_wait_keys=()):
        nc = self.nc
        fw = []
        for k in final_wait_keys:
            t = self.last_w.get(k)
            if t is not None:
                fw.append(t)
        sched = self

        def run(eng_name, eng):
            for item in sched.ops[eng_name]:
                if item[0] == "op":
                    _, fn, waits = item
                    for (s, v) in waits:
                        eng.wait_ge(sched._semof(s), v)
                    ins = fn(eng)
                    ins.then_inc(sched.sem[eng_name], 1)
                elif item[0] == "dmaf":
                    _, fn, waits, i = item
                    for (s, v) in waits:
                        eng.wait_ge(sched._semof(s), v)
                    fn(eng).then_inc(sched.dsems[i], 16)
                else:
                    _, (out, in_, kw), waits, i = item
                    for (s, v) in waits:
                        eng.wait_ge(sched._semof(s), v)
                    eng.dma_start(out=out, in_=in_, **kw).then_inc(sched.dsems[i], 16)
            if eng_name == "sp":
                for (s, v) in fw:
                    eng.wait_ge(sched._semof(s), v)

        with nc.Block() as block:
            @block.tensor
            def _(e):
                run("pe", e)

            @block.scalar
            def _(e):
                run("act", e)

            @block.vector
            def _(e):
                run("dve", e)

            @block.gpsimd
            def _(e):
                run("pool", e)

            @block.sync
            def _(e):
                run("sp", e)


class Arena:
    def __init__(self, S, ap):
        self.S = S
        self.ap = ap
        self.live = []

    def alloc(self, name, at, dtype, free_shape):
        esz = 2 if dtype == BF16 else 4
        nel = int(np.prod(free_shape))
        nbytes = nel * esz
        start, end = at, at + nbytes
        assert start % 4 == 0 and end <= self.ap.shape[1] * 2, (name, start, end)
        olds = [n for (s, e, n) in self.live if s < end and start < e]
        self.live.append((start, end, name))
        if olds:
            self.S.alias(name, set(olds))
        v = self.ap[:, start // 2:end // 2]
        if dtype != BF16:
            v = v.bitcast(dtype)
        if len(free_shape) == 2:
            v = v.rearrange("p (a b) -> p a b", a=free_shape[0])
        elif len(free_shape) == 3:
            v = v.rearrange("p (a b c) -> p a b c", a=free_shape[0], b=free_shape[1])
        return v


class Rot:
    def __init__(self, items):
        self.items = list(items)
        self.i = 0

    def next(self):
        v = self.items[self.i % len(self.items)]
        self.i += 1
        return v


def build(stage="D"):
    nc = bass.Bass("TRN2", target_bir_lowering=False)

    def din(name, shape, dtype=F32):
        return nc.dram_tensor(name, list(shape), dtype, kind="ExternalInput").ap()

    def dout(name, shape, dtype=F32):
        return nc.dram_tensor(name, list(shape), dtype, kind="ExternalOutput").ap()

    xT_all = din("xT_all", [16, 128, 16 * 256])
    w_kv = din("w_kv", [4, 2, 128, 16 * 256])
    xT_own = din("xT_own", [128, 16 * NT])
    x_own = din("x_own", [NT, D])
    w_inA = din("w_inA", [6, 128, 16 * 512])
    w_outT = din("w_outT", [4, 128, 16 * 512])
    w_gu = din("w_gu", [32, 128, 16 * 512])
    w_dn = din("w_dn", [32, 128, 2 * 2048])
    wr_d = din("wr_cat", [D, 36])
    brB_d = din("br_b", [128, 36])
    ln1g_d = din("ln1g_b", [128, D])
    ln1b_d = din("ln1b_b", [128, D])
    ln2g_d = din("ln2g_b", [128, D])
    ln2b_d = din("ln2b_b", [128, D])
    relb_d = din("rel_bias", [32, 8])
    lamp_d = din("lamp_b", [128, 256])
    subg_d = din("subg_b", [128, 128])
    sglng_d = din("sglng_b", [128, 1024])
    sglnb_d = din("sglnb_b", [128, 1024])
    sgwT_d = din("sg_wT", [8, 128, 128])
    sgb_d = din("sg_b_row", [1, 1024])
    E1_d = din("E1", [32, NTYPES * 256])
    E2_d = din("E2", [32, 26])
    ident_d = din("ident", [128, 128])
    uscr = nc.dram_tensor("uscr", [8, NTYPES * 256], F32, kind="Internal").ap()
    NSL = 48
    if MOE_SPARSE:
        tri_d = din("tri", [128, 128])
        iota_d = din("iota_p", [128, 1])
        Xslots = nc.dram_tensor("Xslots", [NSL * 128, D], BF16, kind="Internal").ap()
        Yslots = nc.dram_tensor("Yslots", [NSL * 128, D], BF16, kind="Internal").ap()

    with contextlib.ExitStack() as es:
        S = Sched(nc, es)

        def sb(name, shape, dtype):
            return es.enter_context(nc.sbuf_tensor(name, list(shape), dtype))

        psp = [es.enter_context(nc.psum_tensor("psp%d" % i, [128, 1024], F32)) for i in range(4)]
        ps = [psp[i // 2][:, (i % 2) * 512:(i % 2 + 1) * 512] for i in range(8)]
        PK = ["ps%d" % i for i in range(8)]

        ident_f = sb("ident_f", [128, 128], F32)
        ident_b = sb("ident_b", [128, 128], BF16)
        ones_f = sb("ones_f", [32, 128], F32)
        ones_b = sb("ones_b", [1, 128], BF16)
        lamp = sb("lamp", [128, 256], F32)
        lamw = sb("lamw", [128, 8], F32)
        subg = sb("subg", [128, 128], F32)
        relb = sb("relb", [32, 8], F32)
        E2s = sb("E2s", [32, 26], F32)
        rE2 = sb("rE2", [32, 8, 26], F32)
        cbB = sb("cbB", [128, 8, 26], F32)
        sgb_row = sb("sgb_row", [1, 1024], BF16)
        comb = sb("comb", [128, 8, 32], F32)
        OH = sb("OH", [128, 8, 64], F32)
        W12 = sb("W12", [128, 8, 2], F32)
        QT = sb("QT", [128, 8 * 1024], BF16)
        asT = sb("asT", [128, 16, 1024], BF16)
        arena_t = sb("arena", [128, 65536], BF16)
        AR = Arena(S, arena_t[:, :])
        QTv = QT[:, :].rearrange("p (h t) -> p h t", h=8)
        QA = Arena(S, QT[:, :])
        QA.live.append((0, 16384, "QT"))

        S.dma("sp", ident_f[:], ident_d, writes=["ident_f"])
        S.dma("pool", ident_b[:], ident_d, writes=["ident_b"])
        S.dma("sp", lamp[:], lamp_d, writes=["lamp"])
        S.dma("sp", subg[:], subg_d, writes=["subg"])
        S.dma("sp", relb[:], relb_d, writes=["relb"])
        S.dma("sp", E2s[:], E2_d, writes=["E2s"])
        S.dma("pool", sgb_row[:], sgb_d, writes=["sgb_row"])
        S.op("dve", lambda e: e.memset(ones_f[:], 1.0), writes=["ones_f"])
        S.op("dve", lambda e: e.memset(ones_b[:], 1.0), writes=["ones_b"])
        mhalf = sb("mhalf", [128, 8], F32)
        S.op("dve", lambda e: e.memset(mhalf[:], -0.5), writes=["mhalf"])
        lp = lamp[:, :].rearrange("p (a b) -> p a b", a=4)
        prod = sb("lamprod", [128, 2, 64], F32)
        S.op("dve", lambda e: e.tensor_tensor(prod[:, 0, :], lp[:, 0, :], lp[:, 1, :], ALU.mult),
             reads=["lamp"], writes=["lamprod0"])
        S.op("dve", lambda e: e.tensor_tensor(prod[:, 1, :], lp[:, 2, :], lp[:, 3, :], ALU.mult),
             reads=["lamp"], writes=["lamprod1"])
        S.op("dve", lambda e: e.reduce_sum(lamw[:, 0:2], prod[:, :, :], AX.X),
             reads=["lamprod0", "lamprod1"], writes=["lamw01"])
        S.op("act", lambda e: e.activation(lamw[:, 2:4], lamw[:, 0:2], AF.Exp), reads=["lamw01"], writes=["lamw23"])
        S.op("dve", lambda e: e.tensor_tensor(lamw[:, 4:5], lamw[:, 2:3], lamw[:, 3:4], ALU.subtract),
             reads=["lamw23"], writes=["lamw4"])
        S.op("dve", lambda e: e.tensor_scalar(lamw[:, 5:6], lamw[:, 4:5], -1.0, -LAM_INIT, ALU.mult, ALU.add),
             reads=["lamw4"], writes=["nlam"])
        S.op("dve", lambda e: e.tensor_scalar(subg[:], subg[:], 1.0 - LAM_INIT, None, ALU.mult),
             reads=["subg"], writes=["subg"])
        nlam = lamw[:, 5:6]

        biasT = AR.alloc("biasT", 0, BF16, [8, NTYPES, 128])
        E1s = AR.alloc("E1s", 83968, F32, [NTYPES * 256])
        u_sb = AR.alloc("u_sb", 93184, F32, [NTYPES * 256])
        S.dma("sp", E1s[0:32, :], E1_d, writes=["E1s"])
        ncol = NTYPES * 256
        for ci, c0 in enumerate(range(0, ncol, 512)):
            w = min(512, ncol - c0)
            bk = ci % 4

            def f(e, c0=c0, w=w, bk=bk):
                return e.matmul(ps[bk][0:8, 0:w], relb[:, :], E1s[0:32, c0:c0 + w], start=True, stop=True)
            S.op("pe", f, reads=["relb", "E1s"], writes=[PK[bk]])
            S.op("act", lambda e, c0=c0, w=w, bk=bk: e.activation(u_sb[0:8, c0:c0 + w], ps[bk][0:8, 0:w], AF.Copy, scale=8.0),
                 reads=[PK[bk]], writes=[("u_sb", ci)])
        S.dma("sp", uscr, u_sb[0:8, :], reads=[("u_sb", ci) for ci in range((ncol + 511) // 512)], writes=["uscr"])
        for h in range(8):
            src = bass.AP(tensor=uscr.tensor, offset=h * ncol, ap=[[1, 128], [256, NTYPES], [1, 128]])
            S.dma("pool", biasT[:, h, :, :], src, reads=["uscr"], writes=[("biasT", h)])
        for h in range(8):
            S.op("dve", lambda e, h=h: e.tensor_scalar(rE2[:, h, :], E2s[:, :], relb[:, h:h + 1], None, ALU.mult),
                 reads=["relb", "E2s"], writes=[("rE2", h)])
        S.op("pe", lambda e: e.matmul(ps[4][:, 0:208], ones_f[:, :], rE2[:, :, :].rearrange("p a b -> p (a b)"), start=True, stop=True),
             reads=["ones_f"] + [("rE2", h) for h in range(8)], writes=[PK[4]])
        S.op("dve", lambda e: e.tensor_copy(cbB[:, :, :].rearrange("p a b -> p (a b)"), ps[4][:, 0:208]), reads=[PK[4]], writes=["cbB"])

        if MOE_SPARSE:
            zt = asT[:, 0:2, :].rearrange("p a b -> p (a b)")
            ztk = [("asT", h_, t_) for h_ in range(2) for t_ in range(8)]
            S.op("dve", lambda e: e.memset(zt, 0.0), writes=ztk)
            for j in range(48):
                S.dma("sp", Xslots[j * 128:(j + 1) * 128, :], zt, reads=ztk, writes=["Xzero"])

        xTo = AR.alloc("xTo", 18432, BF16, [16, 1024])
        wA = [AR.alloc("wA0", 51200, BF16, [16, 512]), AR.alloc("wA1", 67584, BF16, [16, 512])]
        uT = AR.alloc("uT", 83968, BF16, [8, 1024])
        lnAg = AR.alloc("lnAg", 100352, F32, [1024])
        lnAb = AR.alloc("lnAb", 104448, F32, [1024])
        sgwT = AR.alloc("sgwT", 108544, BF16, [8, 128])
        vgt = [AR.alloc("vgt0", 110592, F32, [512]), AR.alloc("vgt1", 112640, F32, [512])]
        sqt = AR.alloc("sqt", 114688, F32, [512])
        vnf = AR.alloc("vnf", 116736, F32, [512])
        vnb = [AR.alloc("vnb0", 118784, BF16, [4, 128]), AR.alloc("vnb1", 119808, BF16, [4, 128])]
        stA = AR.alloc("stA", 120832, F32, [2, 32])

        S.dma("pool", xTo.rearrange("p a b -> p (a b)"), xT_own, writes=["xTo"])
        S.dma("sp", lnAg, sglng_d, writes=["lnAg"])
        S.dma("sp", lnAb, sglnb_d, writes=["lnAb"])
        S.dma("pool", sgwT, sgwT_d.rearrange("g q p -> q g p"), writes=["sgwT"])

        rot4 = Rot([0, 1, 2, 3])
        evq = Rot(["act", "dve"])
        sqt2 = [sqt, AR.alloc("sqt1", 121344, F32, [512])]
        vnf2 = [vnf, AR.alloc("vnf1", 123392, F32, [512])]
        gchains = []
        gcount = [0]

        def interleave(chains, width):
            it = iter(chains)
            active = []
            for _ in range(width):
                c = next(it, None)
                if c is not None:
                    active.append(list(c))
            while active:
                for c in list(active):
                    c.pop(0)()
                    if not c:
                        active.remove(c)
                        n = next(it, None)
                        if n is not None:
                            active.append(list(n))

        def g_chain(bi, i, tt, wt, wkey):
            vb = gcount[0] % 2
            gcount[0] += 1
            bkc = [None]
            vg = vgt[vb]
            vgk = "vgt%d" % vb
            sq = sqt2[vb]
            vf = vnf2[vb]
            sfx = "_%d" % vb
            vg3 = vg.rearrange("p (a b) -> p a b", a=4)
            sq3 = sq.rearrange("p (a b) -> p a b", a=4)
            s1, s2, mean, m2 = stA[:, vb, 0:4], stA[:, vb, 4:8], stA[:, vb, 8:12], stA[:, vb, 12:16]
            var, rstd = stA[:, vb, 16:20], stA[:, vb, 20:24]
            vn = vnb[vb]
            vnk = "vnb%d" % vb
            sbk = 4 + vb
            ps3 = ps[sbk][:, :].rearrange("p (a b) -> p a b", a=4)
            st = []

            def s_mm():
                bk = rot4.next()
                bkc[0] = bk

                def f(e):
                    for c in range(16):
                        ins = e.matmul(ps[bk][:, :], xTo[:, c, tt * 128:(tt + 1) * 128], wt[:, c, :],
                                       start=(c == 0), stop=(c == 15))
                    return ins
                S.op("pe", f, reads=[wkey, "xTo"], writes=[PK[bk]])
                S.op("act", lambda e: e.activation(vg, ps[bk][:, :], AF.Gelu_apprx_tanh), reads=[PK[bk]], writes=[vgk])
            st.append(s_mm)
            st.append(lambda: S.op("dve", lambda e: e.reduce_sum(s1, vg3, AX.X), reads=[vgk], writes=["s1" + sfx]))
            st.append(lambda: S.op("dve", lambda e: e.tensor_tensor(sq, vg, vg, ALU.mult), reads=[vgk], writes=["sqt" + sfx]))
            st.append(lambda: S.op("dve", lambda e: e.reduce_sum(s2, sq3, AX.X), reads=["sqt" + sfx], writes=["s2" + sfx]))
            st.append(lambda: S.op("dve", lambda e: e.tensor_scalar(mean, s1, 1.0 / 128, None, ALU.mult), reads=["s1" + sfx], writes=["mean" + sfx]))
            st.append(lambda: S.op("dve", lambda e: e.tensor_tensor(m2, mean, mean, ALU.mult), reads=["mean" + sfx], writes=["m2" + sfx]))
            st.append(lambda: S.op("dve", lambda e: e.scalar_tensor_tensor(var, s2, 1.0 / 128, m2, ALU.mult, ALU.subtract),
                                   reads=["s2" + sfx, "m2" + sfx], writes=["var" + sfx]))
            st.append(lambda: S.op("dve", lambda e: e.tensor_scalar(var, var, EPS, None, ALU.add), reads=["var" + sfx], writes=["var" + sfx]))
            st.append(lambda: S.op("pool", lambda e: e.tensor_tensor(rstd, var, mhalf[:, 0:4], ALU.pow), reads=["var" + sfx, "mhalf"], writes=["rstd" + sfx]))
            for gg in range(4):
                st.append(lambda gg=gg: S.op("dve", lambda e: e.tensor_scalar(
                    vf[:, gg * 128:(gg + 1) * 128], vg[:, gg * 128:(gg + 1) * 128],
                    mean[:, gg:gg + 1], rstd[:, gg:gg + 1], ALU.subtract, ALU.mult),
                    reads=[vgk, "mean" + sfx, "rstd" + sfx], writes=[("vnf" + sfx, gg)]))
            st.append(lambda: S.op("dve", lambda e: e.tensor_tensor(vf, vf, lnAg[:, i * 512:(i + 1) * 512], ALU.mult),
                                   reads=[("vnf" + sfx, g_) for g_ in range(4)] + ["lnAg"], writes=["vnf2" + sfx]))
            st.append(lambda: S.op("dve", lambda e: e.tensor_tensor(vn.rearrange("p a b -> p (a b)"), vf, lnAb[:, i * 512:(i + 1) * 512], ALU.add),
                                   reads=["vnf2" + sfx, "lnAb"], writes=[vnk] + [("vnf" + sfx, g_) for g_ in range(4)]))

            def s_sp():
                def fs(e):
                    for gg in range(4):
                        g = i * 4 + gg
                        e.matmul(ps3[:, gg, :], vn[:, gg, :], sgwT[:, g, :], start=True, stop=False)
                        ins = e.matmul(ps3[:, gg, :], ones_b[0:1, :], sgb_row[0:1, g * 128:(g + 1) * 128],
                                       start=False, stop=True)
                    return ins
                S.op("pe", fs, reads=[vnk, "sgwT", "ones_b", "sgb_row"], writes=[PK[sbk]])
                dst = asT[:, 8 + i * 4:8 + (i + 1) * 4, tt * 128:(tt + 1) * 128]
                usrc = uT[:, i * 4:(i + 1) * 4, tt * 128:(tt + 1) * 128]
                S.op("dve", lambda e: e.tensor_tensor(dst, usrc, ps3, ALU.mult),
                     reads=[PK[sbk]] + [("uT", i * 4 + g_, tt // 4) for g_ in range(4)],
                     writes=[("asT", 8 + i * 4 + g_, tt) for g_ in range(4)])
            st.append(s_sp)
            return st

        blocks = [("Q", 0, 0), ("Q", 1, 512), ("U", 0, 3072), ("U", 1, 3584), ("G", 0, 4096), ("G", 1, 4608)]
        kA = 0
        for bi, (kind, i, c0) in enumerate(blocks):
            buf = bi % 2
            wkey = "wA%d" % buf
            wt = wA[buf]
            S.dma("pool", wt.rearrange("p a b -> p (a b)"), w_inA[bi], writes=[wkey])
            if kind in ("Q", "U"):
                for hh in range(4):
                    h = i * 4 + hh
                    for th in range(2):
                        bk = rot4.next()

                        def f(e, wt=wt, hh=hh, th=th, bk=bk):
                            for c in range(16):
                                ins = e.matmul(ps[bk][:, :], wt[:, c, hh * 128:(hh + 1) * 128],
                                               xTo[:, c, th * 512:(th + 1) * 512], start=(c == 0), stop=(c == 15))
                            return ins
                        S.op("pe", f, reads=[wkey, "xTo"], writes=[PK[bk]])
                        if kind == "Q":
                            q = evq.next()
                            dst = QTv[:, h, th * 512:(th + 1) * 512]
                            if q == "act":
                                S.op("act", lambda e, dst=dst, bk=bk: e.activation(dst, ps[bk][:, :], AF.Copy),
                                     reads=[PK[bk]], writes=[("QT", h, th)])
                            else:
                                S.op("dve", lambda e, dst=dst, bk=bk: e.tensor_copy(dst, ps[bk][:, :]),
                                     reads=[PK[bk]], writes=[("QT", h, th)])
                        else:
                            dst = uT[:, h, th * 512:(th + 1) * 512]
                            S.op("act", lambda e, dst=dst, bk=bk: e.activation(dst, ps[bk][:, :], AF.Gelu_apprx_tanh),
                                 reads=[PK[bk]], writes=[("uT", h, th)])
            else:
                for tt in range(8):
                    gchains.append(g_chain(bi, i, tt, wt, wkey))
        interleave(gchains, 2)

        outs = []

        def dump(name, src_ap, shape, dtype, reads):
            o = dout(name, shape, dtype)
            S.dma("sp", o, src_ap, reads=reads, writes=[name])
            outs.append(name)

        if stage == "A":
            dump("dbg_sT", asT[:, 8:16, :], [128, 8, 1024], BF16,
                 [("asT", 8 + g, tt) for g in range(8) for tt in range(8)])
            dump("dbg_QT", QTv, [128, 8, 1024], BF16, [("QT", h, th) for h in range(8) for th in range(2)])
            dump("dbg_bias", biasT, [128, 8, NTYPES, 128], BF16, [("biasT", h) for h in range(8)])
            dump("dbg_cb", cbB[:, :, :], [128, 8, 26], F32, ["cbB"])
            dump("dbg_lam", lamw[:, :], [128, 8], F32, ["nlam"])
            S.emit(final_wait_keys=outs)
            return nc

        KT = [AR.alloc("KT0", 18432, BF16, [2, 4096]), AR.alloc("KT1", 34816, BF16, [2, 4096])]
        V = [AR.alloc("V0", 51200, BF16, [32, 2, 130]), AR.alloc("V1", 67840, BF16, [32, 2, 130])]
        wk = AR.alloc("wk", 84480, BF16, [16, 256])
        wv = AR.alloc("wv", 92672, BF16, [16, 256])
        xtb = [AR.alloc("xt0", 100864, BF16, [16, 256]), AR.alloc("xt1", 109056, BF16, [16, 256])]
        PT = [AR.alloc("PT%d" % k, 117248 + k * 1024, BF16, [2, 256]) for k in range(3)]
        accS = [AR.alloc("accS%d" % k, 120320 + k * 2080, F32, [4, 130]) for k in range(2)]
        o_f = AR.alloc("o_f", 124480, F32, [2, 128])
        sq_f = AR.alloc("sq_f", 125504, F32, [2, 128])
        a_bf = [AR.alloc("a_bf%d" % k, 126528 + k * 512, BF16, [2, 128]) for k in range(2)]
        stB = AR.alloc("stB", 127552, F32, [64])

        for vb in range(2):
            S.op("dve", lambda e, vb=vb: e.memset(V[vb][:, :, :, 128:129], 1.0), writes=["Vones%d" % vb])

        def near_tiles(qg4):
            return [j for j in range(max(0, 2 * qg4 - 1), 2 * qg4 + 3)] + ([31] if qg4 == 0 else [])

        def btype(i, j):
            if j <= 7:
                d = j - i
                return 0 if d == 0 else 1 if d == 1 else 2 if d == -1 else 3 if d >= 2 else 4
            if j == 8:
                return 5 if i == 7 else 6
            assert j == 31
            return 7 if i == 0 else 8

        def far_col(qg4, j):
            if j <= 7:
                return 25 if j > 2 * qg4 + 1 else 24
            return j - 8

        kvstate = {"x": 0}

        def kv_chunks(hg, banks, evs):
            vb = hg % 2
            ktk, vk = "KT%d" % vb, "V%d" % vb
            chunks = []

            def xdma(T):
                xb = (hg * 16 + T) % 2
                S.dma("pool", xtb[xb].rearrange("p a b -> p (a b)"), xT_all[T], writes=["xt%d" % xb])

            def mk(T, kind, idx):
                stt = {}
                xb = (hg * 16 + T) % 2
                xt = xtb[xb]
                xk = "xt%d" % xb

                def quarter(qi):
                    def run():
                        if qi == 0:
                            if T == 0 and kind == "K" and idx == 0:
                                S.dma("pool", wk.rearrange("p a b -> p (a b)"), w_kv[hg, 0], writes=["wk"])
                                S.dma("pool", wv.rearrange("p a b -> p (a b)"), w_kv[hg, 1], writes=["wv"])
                                xdma(0)
                            if kind == "K" and idx == 0 and T + 1 < 16:
                                xdma(T + 1)
                            stt["bk"] = banks.next()
                            stt["q"] = evs.next()
                        bk = stt["bk"]
                        c0 = qi * 4
                        if kind == "K":
                            def f(e):
                                for c in range(c0, c0 + 4):
                                    ins = e.matmul(ps[bk][:, 0:256], wk[:, c, idx * 128:(idx + 1) * 128], xt[:, c, :],
                                                   start=(c == 0), stop=(c == 15))
                                return ins
                            S.op("pe", f, reads=["wk", xk], writes=[PK[bk]])
                        else:
                            def f(e):
                                for c in range(c0, c0 + 4):
                                    ins = e.matmul(ps[bk][:, 0:256], xt[:, c, idx * 128:(idx + 1) * 128], wv[:, c, :],
                                                   start=(c == 0), stop=(c == 15))
                                return ins
                            S.op("pe", f, reads=["wv", xk], writes=[PK[bk]])
                        if qi == 3:
                            if kind == "K":
                                dst = KT[vb][:, idx, T * 256:(T + 1) * 256]
                                src_ = ps[bk][:, 0:256]
                                wkeys = [(ktk, idx, T // 2)]
                            else:
                                j = T * 2 + idx
                                dst = V[vb][:, j, :, 0:128]
                                src_ = ps[bk][:, 0:256].rearrange("p (a b) -> p a b", a=2)
                                wkeys = [(vk, j)]
                            if stt["q"] == "act":
                                S.op("act", lambda e: e.activation(dst, src_, AF.Copy), reads=[PK[bk], "Vones%d" % vb], writes=wkeys)
                            else:
                                S.op("dve", lambda e: e.tensor_copy(dst, src_), reads=[PK[bk], "Vones%d" % vb], writes=wkeys)
                    return run
                return [quarter(qi) for qi in range(4)]
            for T in range(16):
                for hl in range(2):
                    chunks.extend(mk(T, "K", hl))
                for st in range(2):
                    chunks.extend(mk(T, "V", st))
            return chunks

        for ch in kv_chunks(0, Rot([0, 1, 2, 3, 4, 5, 6, 7]), Rot(["act", "dve"])):
            ch()

        ps6b = ps[6][:, :].bitcast(BF16)
        stepno = 0
        grp = 0
        deferred = []

        def accv(idx):
            return (4, idx * 130) if idx < 3 else (5, 0)

        for hg in range(4):
            vb = hg % 2
            ktk, vk = "KT%d" % vb, "V%d" % vb
            pend = kv_chunks(hg + 1, Rot([7]), Rot(["dve"])) if hg < 3 else []
            for hl in range(2):
                h = hg * 2 + hl
                for qg4 in range(4):
                    near = near_tiles(qg4)

                    def pv(j, hl=hl, vb=vb, vk=vk):
                        pb = j % 3

                        def f(e):
                            for idx in range(4):
                                m, ib = divmod(idx, 2)
                                bkk, col = accv(idx)
                                ins = e.matmul(ps[bkk][:, col:col + 129], PT[pb][:, m, ib * 128:(ib + 1) * 128],
                                               V[vb][:, j, hl, 0:129], start=(j == 0 and idx in (0, 3)), stop=(j == 31),
                                               skip_group_check=True)
                            return ins
                        S.op("pe", f, reads=["PT%d" % pb, (vk, j)], writes=[PK[4], PK[5]])

                    for j in range(32):
                        sk = stepno % 2
                        stepno += 1
                        b0, b1 = 2 * sk, 2 * sk + 1
                        isnear = j in near

                        def fq(e, j=j, b0=b0, b1=b1, hl=hl, h=h, qg4=qg4, isnear=isnear, vb=vb):
                            for m, bk in ((0, b0), (1, b1)):
                                ins = e.matmul(ps[bk][:, 0:256], KT[vb][64 * m:64 * m + 64, hl, j * 128:(j + 1) * 128],
                                               QTv[64 * m:64 * m + 64, h, qg4 * 256:(qg4 + 1) * 256],
                                               start=True, stop=True, tile_position=(64 * m, 0))
                            if isnear:
                                for m, bk in ((0, b0), (1, b1)):
                                    for ib in range(2):
                                        ty = btype(2 * qg4 + ib, j)
                                        ins = e.matmul(ps[bk][:, ib * 128:(ib + 1) * 128], ident_b[:, :], biasT[:, h, ty, :],
                                                       start=False, stop=True, skip_group_check=True)
                            return ins
                        S.op("pe", fq, reads=[(ktk, hl, j // 4), ("QT", h, qg4 // 2), ("biasT", h), "ident_b"], writes=[PK[b0], PK[b1]])
                        pb = j % 3
                        src2 = psp[sk][:, :].rearrange("p (b n) -> p b n", b=2)[:, :, 0:256]
                        if isnear:
                            S.op("act", lambda e, pb=pb, src2=src2: e.activation(PT[pb], src2, AF.Exp, scale=0.125),
                                 reads=[PK[b0], PK[b1]], writes=["PT%d" % pb])
                        else:
                            fc = far_col(qg4, j)
                            S.op("act", lambda e, pb=pb, src2=src2, fc=fc, h=h: e.activation(
                                PT[pb], src2, AF.Exp, bias=cbB[:, h, fc:fc + 1], scale=0.125),
                                reads=[PK[b0], PK[b1], "cbB"], writes=["PT%d" % pb])
                        if j >= 1:
                            pv(j - 1)
                        if j == 6:
                            for d_ in deferred:
                                d_()
                            deferred.clear()
                        if pend:
                            pend.pop(0)()
                    pv(31)
                    ab = grp % 2
                    grp += 1
                    aS = accS[ab]
                    ask = "accS%d" % ab
                    S.op("dve", lambda e, aS=aS: e.tensor_copy(aS[:, 0:3, :].rearrange("p a b -> p (a b)"), ps[4][:, 0:390]),
                         reads=[PK[4]], writes=[(ask, 0)])
                    S.op("dve", lambda e, aS=aS: e.tensor_copy(aS[:, 3, :], ps[5][:, 0:130]), reads=[PK[5]], writes=[(ask, 1)])
                    rec = stB[:, 0:4]
                    nl = stB[:, 4:6]
                    ss = stB[:, 6:8]
                    rs = stB[:, 8:10]
                    S.op("dve", lambda e, aS=aS: e.reciprocal(stB[:, 0:4].rearrange("p (a b) -> p a b", b=1), aS[:, :, 128:129]),
                         reads=[(ask, 0), (ask, 1)], writes=["rec"])
                    S.op("dve", lambda e: e.tensor_scalar(nl, rec[:, 2:4], nlam, None, ALU.mult), reads=["rec", "nlam"], writes=["nl"])
                    for ib in range(2):
                        S.op("dve", lambda e, ib=ib, aS=aS: e.tensor_scalar(sq_f[:, ib, :], aS[:, 2 + ib, 0:128], nl[:, ib:ib + 1], None, ALU.mult),
                             reads=[(ask, 0), (ask, 1), "nl"], writes=[("sq_f", ib)])
                        S.op("dve", lambda e, ib=ib, aS=aS: e.scalar_tensor_tensor(o_f[:, ib, :], aS[:, ib, 0:128], rec[:, ib:ib + 1], sq_f[:, ib, :], ALU.mult, ALU.add),
                             reads=[(ask, 0), "rec", ("sq_f", ib)], writes=[("o_f", ib)])
                    S.op("dve", lambda e: e.tensor_tensor(sq_f.rearrange("p a b -> p (a b)"), o_f.rearrange("p a b -> p (a b)"), o_f.rearrange("p a b -> p (a b)"), ALU.mult),
                         reads=[("o_f", 0), ("o_f", 1)], writes=[("sq_f", 0), ("sq_f", 1)])
                    S.op("dve", lambda e: e.reduce_sum(ss, sq_f, AX.X), reads=[("sq_f", 0), ("sq_f", 1)], writes=["ss"])
                    S.op("dve", lambda e: e.tensor_scalar(rs, ss, 1.0 / 128, EPS, ALU.mult, ALU.add), reads=["ss"], writes=["rs0"])
                    S.op("pool", lambda e: e.tensor_tensor(rs, rs, mhalf[:, 0:2], ALU.pow), reads=["rs0", "mhalf"], writes=["rs"])
                    abuf = a_bf[ab]
                    abk = "a_bf%d" % ab
                    for ib in range(2):
                        S.op("dve", lambda e, ib=ib, abuf=abuf: e.scalar_tensor_tensor(abuf[:, ib, :], o_f[:, ib, :], rs[:, ib:ib + 1], subg[:, :], ALU.mult, ALU.mult),
                             reads=[("o_f", ib), "rs", "subg"], writes=[(abk, ib)])

                    def fin(abuf=abuf, abk=abk, h=h, qg4=qg4):
                        def ft(e):
                            for ib in range(2):
                                ins = e.transpose(ps6b[:, ib * 128:(ib + 1) * 128], abuf[:, ib, :], ident_b[:, :])
                            return ins
                        S.op("pe", ft, reads=[(abk, 0), (abk, 1), "ident_b"], writes=[PK[6]])
                        S.op("dve", lambda e: e.tensor_copy(asT[:, h, qg4 * 256:(qg4 + 1) * 256], ps6b[:, 0:256]),
                             reads=[PK[6]], writes=[("asT", h, 2 * qg4), ("asT", h, 2 * qg4 + 1)])
                    deferred.append(fin)
            while pend:
                pend.pop(0)()
        for d_ in deferred:
            d_()
        deferred.clear()

        if stage == "B":
            dump("dbg_aT", asT[:, 0:8, :], [128, 8, 1024], BF16, [("asT", h, tt) for h in range(8) for tt in range(8)])
            S.emit(final_wait_keys=outs)
            return nc

        z = AR.alloc("z", 0, F32, [8, 2048])
        wo = [AR.alloc("wo0", 65536, BF16, [16, 512]), AR.alloc("wo1", 81920, BF16, [16, 512])]
        xo = [AR.alloc("xo0", 98304, F32, [512]), AR.alloc("xo1", 100352, F32, [512])]
        lng = AR.alloc("lnCg", 102400, F32, [2048])
        lnb = AR.alloc("lnCb", 110592, F32, [2048])
        h1Tf = AR.alloc("h1Tf", 118784, F32, [16, 128])
        wr = QA.alloc("wr", 0, F32, [16, 36])
        brB = QA.alloc("brB", 2304, F32, [36])
        h1T = asT
        h1Tf2 = [h1Tf, QA.alloc("h1Tf1", 4096, F32, [16, 128])]

        S.dma("sp", wr, wr_d.rearrange("(c p) n -> p c n", p=128), writes=["wr"])
        S.dma("sp", brB, brB_d, writes=["brB"])
        S.dma("sp", lng, ln1g_d, writes=["lnCg"])
        S.dma("sp", lnb, ln1b_d, writes=["lnCb"])
        kxo = 0
        for db in range(4):
            wb = db % 2
            S.dma("pool", wo[wb].rearrange("p a b -> p (a b)"), w_outT[db], writes=["wo%d" % wb])
            for tt in range(8):
                xb = kxo % 2
                kxo += 1
                S.dma("sp", xo[xb], x_own[tt * 128:(tt + 1) * 128, db * 512:(db + 1) * 512], writes=["xo%d" % xb])
                bk = rot4.next()

                def f(e, wb=wb, tt=tt, bk=bk):
                    for c in range(16):
                        ins = e.matmul(ps[bk][:, :], asT[:, c, tt * 128:(tt + 1) * 128], wo[wb][:, c, :],
                                       start=(c == 0), stop=(c == 15))
                    return ins
                S.op("pe", f, reads=["wo%d" % wb] + [("asT", c, tt) for c in range(16)], writes=[PK[bk]])
                S.op("dve", lambda e, xb=xb, tt=tt, db=db, bk=bk: e.scalar_tensor_tensor(
                    z[:, tt, db * 512:(db + 1) * 512], xo[xb], ALPHA, ps[bk][:, :], ALU.mult, ALU.add),
                    reads=["xo%d" % xb, PK[bk]], writes=[("z", tt, db)])
        S.alias("h1T", {"asT"})

        tmpC = []
        for par in range(2):
            base = 126976 + par * 1280
            tmpC.append(dict(
                st=AR.alloc("stC%d" % par, base, F32, [64]),
                Lg=AR.alloc("Lg%d" % par, base + 256, F32, [36]),
                em=AR.alloc("em%d" % par, base + 400, F32, [32]),
                em2=AR.alloc("em2%d" % par, base + 528, F32, [32]),
                oh1=AR.alloc("oh1%d" % par, base + 656, F32, [32]),
                oh2=AR.alloc("oh2%d" % par, base + 784, F32, [32]),
                c1t=AR.alloc("c1t%d" % par, base + 912, F32, [32]),
                rt=AR.alloc("rt%d" % par, base + 1040, F32, [32]),
            ))

        class Chain:
            def __init__(self):
                self.st = []

            def add(self, eng, fn, reads=(), writes=()):
                self.st.append(lambda: S.op(eng, fn, reads=list(reads), writes=list(writes)))

        def ln_stages(ch, tt, stt, lng_, lnb_, gk, bk_, sfx):
            st6 = stt[:, 0:24].rearrange("p (a b) -> p a b", a=4)
            mv = stt[:, 24:26]
            rstd = stt[:, 26:27]
            zk = [("z", tt, db) for db in range(4)]
            for db in range(4):
                ch.add("dve", lambda e, db=db: e.bn_stats(st6[:, db, :], z[:, tt, db * 512:(db + 1) * 512]),
                       reads=[("z", tt, db)], writes=[("st6" + sfx, db)])
            ch.add("dve", lambda e: e.bn_aggr(mv, st6), reads=[("st6" + sfx, db) for db in range(4)], writes=["mv" + sfx])
            ch.add("dve", lambda e: e.tensor_scalar(rstd, mv[:, 1:2], EPS, None, ALU.add), reads=["mv" + sfx], writes=["rstd0" + sfx])
            ch.add("pool", lambda e: e.tensor_tensor(rstd, rstd, mhalf[:, 0:1], ALU.pow), reads=["rstd0" + sfx, "mhalf"], writes=["rstd" + sfx])
            ch.add("dve", lambda e: e.tensor_scalar(z[:, tt, :], z[:, tt, :], mv[:, 0:1], rstd, ALU.subtract, ALU.mult),
                   reads=zk + ["mv" + sfx, "rstd" + sfx], writes=zk)
            ch.add("dve", lambda e: e.tensor_tensor(z[:, tt, :], z[:, tt, :], lng_, ALU.mult), reads=zk + [gk], writes=zk)
            ch.add("dve", lambda e: e.tensor_tensor(z[:, tt, :], z[:, tt, :], lnb_, ALU.add), reads=zk + [bk_], writes=zk)
            return zk

        trot = Rot([4, 5])

        def c_chain(tt):
            par = tt % 2
            tm = tmpC[par]
            sfx = "_c%d" % par
            stt, Lg, em, em2, oh1, oh2, c1t, rt = tm["st"], tm["Lg"], tm["em"], tm["em2"], tm["oh1"], tm["oh2"], tm["c1t"], tm["rt"]
            ch = Chain()
            h1Tf = h1Tf2[par]
            hk = "h1Tf%d" % par
            rb = 6 + par
            zk = ln_stages(ch, tt, stt, lng, lnb, "lnCg", "lnCb", sfx)
            for c4 in range(4):
                def tr(c4=c4):
                    bk = trot.next()

                    def f(e):
                        for k in range(4):
                            c = c4 * 4 + k
                            ins = e.transpose(ps[bk][:, k * 128:(k + 1) * 128], z[:, tt, c * 128:(c + 1) * 128], ident_f[:, :])
                        return ins
                    S.op("pe", f, reads=zk + ["ident_f"], writes=[PK[bk]])
                    src3 = ps[bk][:, :].rearrange("p (a b) -> p a b", a=4)
                    S.op("dve", lambda e: e.tensor_copy(h1Tf[:, c4 * 4:(c4 + 1) * 4, :], src3), reads=[PK[bk]], writes=[(hk, c4)])
                    S.op("act", lambda e: e.activation(h1T[:, c4 * 4:(c4 + 1) * 4, tt * 128:(tt + 1) * 128], h1Tf[:, c4 * 4:(c4 + 1) * 4, :], AF.Copy),
                         reads=[(hk, c4)], writes=[("h1T", c4, tt)])
                ch.st.append(tr)
            ch.add("act", lambda e: e.activation(z[:, tt, :], z[:, tt, :], AF.Copy, scale=ALPHA), reads=zk, writes=zk)

            def fr(e):
                for c in range(16):
                    ins = e.matmul(ps[rb][:, 0:36], h1Tf[:, c, :], wr[:, c, :], start=(c == 0), stop=(c == 15))
                return ins
            ch.add("pe", fr, reads=[(hk, c4) for c4 in range(4)] + ["wr"], writes=[PK[rb]])
            gm, ngm, gs, gg_, v1, v2, dd, ed, w1, w2 = [stt[:, 32 + k:33 + k] for k in range(10)]
            goh, gex, pen = rt[:, 0:4], rt[:, 4:8], rt[:, 8:12]
            K_ = lambda n: n + sfx
            ch.add("dve", lambda e: e.tensor_tensor(Lg, ps[rb][:, 0:36], brB, ALU.add), reads=[PK[rb], "brB"], writes=[K_("Lg")])
            ch.add("dve", lambda e: e.reduce_max(gm, Lg[:, 0:4], AX.X), reads=[K_("Lg")], writes=[K_("gm")])
            ch.add("dve", lambda e: e.tensor_scalar(goh, Lg[:, 0:4], gm, None, ALU.is_equal), reads=[K_("Lg"), K_("gm")], writes=[K_("goh")])
            ch.add("dve", lambda e: e.tensor_scalar(ngm, gm, -1.0, None, ALU.mult), reads=[K_("gm")], writes=[K_("ngm")])
            ch.add("act", lambda e: e.activation(gex, Lg[:, 0:4], AF.Exp, bias=ngm), reads=[K_("Lg"), K_("ngm")], writes=[K_("gex")])
            ch.add("dve", lambda e: e.reduce_sum(gs, gex, AX.X), reads=[K_("gex")], writes=[K_("gs")])
            ch.add("dve", lambda e: e.reciprocal(gg_, gs), reads=[K_("gs")], writes=[K_("gg")])
            ch.add("dve", lambda e: e.tensor_scalar(pen, goh, -1.0, 1e30, ALU.add, ALU.mult), reads=[K_("goh")], writes=[K_("pen")])
            for g in range(4):
                ch.add("dve", lambda e, g=g: e.tensor_scalar(em[:, g * 8:(g + 1) * 8], Lg[:, 4 + g * 8:12 + g * 8], pen[:, g:g + 1], None, ALU.add),
                       reads=[K_("Lg"), K_("pen")], writes=[(K_("em"), g)])
            emk = [(K_("em"), g) for g in range(4)]
            ch.add("dve", lambda e: e.reduce_max(v1, em, AX.X), reads=emk, writes=[K_("v1")])
            ch.add("dve", lambda e: e.tensor_scalar(oh1, em, v1, None, ALU.is_equal), reads=emk + [K_("v1")], writes=[K_("oh1")])
            ch.add("dve", lambda e: e.scalar_tensor_tensor(em2, oh1, -1e30, em, ALU.mult, ALU.add), reads=emk + [K_("oh1")], writes=[K_("em2")])
            ch.add("dve", lambda e: e.reduce_max(v2, em2, AX.X), reads=[K_("em2")], writes=[K_("v2")])
            ch.add("dve", lambda e: e.tensor_scalar(oh2, em2, v2, None, ALU.is_equal), reads=[K_("em2"), K_("v2")], writes=[K_("oh2")])
            ch.add("dve", lambda e: e.tensor_tensor(dd, v2, v1, ALU.subtract), reads=[K_("v1"), K_("v2")], writes=[K_("dd")])
            ch.add("act", lambda e: e.activation(ed, dd, AF.Exp), reads=[K_("dd")], writes=[K_("ed")])
            ch.add("dve", lambda e: e.tensor_scalar(w1, ed, 1.0, None, ALU.add), reads=[K_("ed")], writes=[K_("den")])
            ch.add("dve", lambda e: e.reciprocal(w1, w1), reads=[K_("den")], writes=[K_("w1a")])
            ch.add("dve", lambda e: e.tensor_tensor(w1, w1, gg_, ALU.mult), reads=[K_("w1a"), K_("gg")], writes=[K_("w1")])
            ch.add("dve", lambda e: e.tensor_tensor(w2, w1, ed, ALU.mult), reads=[K_("w1"), K_("ed")], writes=[K_("w2")])
            ch.add("dve", lambda e: e.tensor_scalar(c1t, oh1, w1, None, ALU.mult), reads=[K_("oh1"), K_("w1")], writes=[K_("c1t")])
            ch.add("dve", lambda e: e.scalar_tensor_tensor(comb[:, tt, :], oh2, w2, c1t, ALU.mult, ALU.add),
                   reads=[K_("oh2"), K_("w2"), K_("c1t")], writes=[("comb", tt)])
            ch.add("dve", lambda e: e.tensor_copy(OH[:, tt, 0:32], oh1), reads=[K_("oh1")], writes=[("OH1", tt)])
            ch.add("dve", lambda e: e.tensor_copy(OH[:, tt, 32:64], oh2), reads=[K_("oh2")], writes=[("OH2", tt)])
            ch.add("dve", lambda e: e.tensor_copy(W12[:, tt, 0:1], w1), reads=[K_("w1")], writes=[("W1", tt)])
            ch.add("dve", lambda e: e.tensor_copy(W12[:, tt, 1:2], w2), reads=[K_("w2")], writes=[("W2", tt)])
            return ch.st

        interleave([c_chain(tt) for tt in range(8)], 2)

        if stage == "C":
            o = dout("dbg_h1a", [NT, D], F32)
            for tt in range(8):
                S.dma("sp", o[tt * 128:(tt + 1) * 128, :], z[:, tt, :], reads=[("z", tt, db) for db in range(4)], writes=[("dbg_h1a", tt)])
                outs.append(("dbg_h1a", tt))
            dump("dbg_comb", comb[:, :, :], [128, 8, 32], F32, [("comb", tt) for tt in range(8)])
            dump("dbg_h1T", h1T[:, :, :], [128, 16, 1024], BF16, [("h1T", c4, tt) for c4 in range(4) for tt in range(8)])
            S.emit(final_wait_keys=outs)
            return nc

        if MOE_SPARSE:
            I32 = mybir.dt.int32
            wgu = [AR.alloc("wgu0", 65536, BF16, [16, 512]), AR.alloc("wgu1", 81920, BF16, [16, 512])]
            wdn = [AR.alloc("wdn%d" % k, 98304 + k * 8192, BF16, [2, 2048]) for k in range(3)]
            HA = Arena(S, asT[:, :, :].rearrange("p a b -> p (a b)"))
            HA.live.append((0, 32768, "asT"))
            HA.live.append((0, 32768, "h1T"))
            xs = [HA.alloc("xs0", 0, BF16, [2048]), HA.alloc("xs1", 4096, BF16, [2048])]
            xsT = [HA.alloc("xsT0", 8192, BF16, [16, 128]), HA.alloc("xsT1", 12288, BF16, [16, 128])]
            ys = [HA.alloc("ys0", 16384, BF16, [2048]), HA.alloc("ys1", 20480, BF16, [2048])]
            sgt1 = [HA.alloc("sgt1_%d" % k, 24576 + k * 1024, F32, [256]) for k in range(3)]
            actb1 = [HA.alloc("actb1_%d" % k, 27648 + k * 512, BF16, [256]) for k in range(3)]
            actT1 = [HA.alloc("actT1_%d" % k, 29184 + k * 512, BF16, [2, 128]) for k in range(3)]
            stD = QA.alloc("stD", 12288, F32, [64])
            A_b = QA.alloc("A_b", 4096, BF16, [8, 32])
            R_sb = QA.alloc("R_sb", 4608, F32, [8, 32])
            n_sb = QA.alloc("n_sb", 5632, F32, [32])
            nt_sb = QA.alloc("nt_sb", 5760, F32, [32])
            cs = [QA.alloc("cs0", 5888, F32, [32]), QA.alloc("cs1", 6016, F32, [32])]
            tb_sb = QA.alloc("tb_sb", 6144, F32, [32])
            G_sb = QA.alloc("G_sb", 6272, F32, [32])
            tmp32 = QA.alloc("tmp32", 6400, F32, [32])
            ej_f = QA.alloc("ej_f", 6528, F32, [NSL])
            ej_i = QA.alloc("ej_i", 6720, I32, [NSL])
            sl_f = QA.alloc("sl_f", 6912, F32, [8, 2])
            sl_i = QA.alloc("sl_i", 6976, I32, [8, 2])
            tri_b = QA.alloc("tri_b", 7040, BF16, [128])
            one_b = QA.alloc("one_b", 7296, BF16, [128])

            S.dma("pool", tri_b, tri_d, writes=["tri_b"])
            iota_p = QA.alloc("iota_p", 7552, F32, [1])
            S.dma("sp", iota_p, iota_d, writes=["iota_p"])
            S.op("dve", lambda e: e.memset(one_b, 1.0), writes=["one_b"])
            ohk = [("OH1", tt) for tt in range(8)] + [("OH2", tt) for tt in range(8)]
            S.op("dve", lambda e: e.tensor_tensor(A_b, OH[:, :, 0:32], OH[:, :, 32:64], ALU.add), reads=ohk, writes=["A_b"])
            for tt in range(9):
                bk = tt % 4

                def f(e, tt=tt, bk=bk):
                    if tt < 8:
                        mm = [(one_b, t2) for t2 in range(tt)] + [(tri_b, tt)]
                    else:
                        mm = [(one_b, t2) for t2 in range(8)]
                    for n_, (lh, t2) in enumerate(mm):
                        ins = e.matmul(ps[bk][:, 0:32], lh, A_b[:, t2, :], start=(n_ == 0), stop=(n_ == len(mm) - 1))
                    return ins
                S.op("pe", f, reads=["A_b", "tri_b", "one_b"], writes=[PK[bk]])
                if tt < 8:
                    S.op("dve", lambda e, tt=tt, bk=bk: e.tensor_copy(R_sb[:, tt, :], ps[bk][:, 0:32]), reads=[PK[bk]], writes=[("R_sb", tt)])
                else:
                    S.op("dve", lambda e, bk=bk: e.tensor_copy(n_sb, ps[bk][:, 0:32]), reads=[PK[bk]], writes=["n_sb"])
            S.op("dve", lambda e: e.tensor_scalar(nt_sb, n_sb, 0.0, None, ALU.is_gt), reads=["n_sb"], writes=["nt_sb"])
            for k in range(1, 8):
                S.op("dve", lambda e, k=k: e.tensor_scalar(tmp32, n_sb, 128.0 * k, None, ALU.is_gt), reads=["n_sb"], writes=["tmp32"])
                S.op("dve", lambda e: e.tensor_tensor(nt_sb, nt_sb, tmp32, ALU.add), reads=["nt_sb", "tmp32"], writes=["nt_sb"])
            S.op("dve", lambda e: e.tensor_copy(cs[0], nt_sb), reads=["nt_sb"], writes=["cs0"])
            cur = 0
            for sh in (1, 2, 4, 8, 16):
                a_, b_ = cs[cur], cs[1 - cur]
                ka, kb = "cs%d" % cur, "cs%d" % (1 - cur)
                S.op("dve", lambda e, a_=a_, b_=b_, sh=sh: e.tensor_copy(b_[:, 0:sh], a_[:, 0:sh]), reads=[ka], writes=[(kb, 0)])
                S.op("dve", lambda e, a_=a_, b_=b_, sh=sh: e.tensor_tensor(b_[:, sh:32], a_[:, sh:32], a_[:, 0:32 - sh], ALU.add),
                     reads=[ka, (ka, 0), (ka, 1)], writes=[(kb, 1), kb])
                cur = 1 - cur
            te_sb = cs[cur]
            tek = "cs%d" % cur
            S.op("dve", lambda e: e.tensor_tensor(tb_sb, te_sb, nt_sb, ALU.subtract), reads=[tek, "nt_sb"], writes=["tb_sb"])
            for j in range(NSL):
                S.op("dve", lambda e, j=j: e.tensor_scalar(tmp32, te_sb, float(j), None, ALU.is_le), reads=[tek], writes=["tmp32"])
                S.op("dve", lambda e, j=j: e.reduce_sum(ej_f[:, j:j + 1], tmp32, AX.X), reads=["tmp32"], writes=[("ej_f", j)])
            S.op("dve", lambda e: e.tensor_scalar(ej_f, ej_f, 128.0, iota_p, ALU.mult, ALU.add), reads=[("ej_f", j) for j in range(NSL)] + ["iota_p"], writes=["ej_f2"])
            S.op("dve", lambda e: e.tensor_copy(ej_i, ej_f), reads=["ej_f2"], writes=["ej_i"])
            for tt in range(8):
                S.op("dve", lambda e, tt=tt: e.scalar_tensor_tensor(G_sb, tb_sb, 128.0, R_sb[:, tt, :], ALU.mult, ALU.add),
                     reads=["tb_sb", ("R_sb", tt)], writes=["G_sb"])
                for c_ in range(2):
                    S.op("dve", lambda e, tt=tt, c_=c_: e.tensor_tensor(tmp32, G_sb, OH[:, tt, 32 * c_:32 * c_ + 32], ALU.mult),
                         reads=["G_sb"] + ohk, writes=["tmp32"])
                    S.op("dve", lambda e, tt=tt, c_=c_: e.reduce_sum(sl_f[:, tt, c_:c_ + 1], tmp32, AX.X), reads=["tmp32"], writes=[("sl_f", tt, c_)])
            S.op("dve", lambda e: e.tensor_copy(sl_i.rearrange("p a b -> p (a b)"), sl_f.rearrange("p a b -> p (a b)")),
                 reads=[("sl_f", tt, c_) for tt in range(8) for c_ in range(2)], writes=["sl_i"])
            if stage == "M":
                dump("dbg_slf", sl_f, [128, 8, 2], F32, [("sl_f", tt, c_) for tt in range(8) for c_ in range(2)])
                dump("dbg_sli", sl_i, [128, 8, 2], I32, ["sl_i"])
                dump("dbg_ejf", ej_f, [128, NSL], F32, ["ej_f2"])
                dump("dbg_eji", ej_i, [128, NSL], I32, ["ej_i"])
                dump("dbg_n", n_sb, [128, 32], F32, ["n_sb"])
                dump("dbg_R", R_sb, [128, 8, 32], F32, [("R_sb", tt) for tt in range(8)])
                dump("dbg_OH", OH[:, :, :], [128, 8, 64], F32, ohk)
                dump("dbg_tb", tb_sb, [128, 32], F32, ["tb_sb"])
                S.emit(final_wait_keys=outs)
                return nc
            h1b = HA.alloc("h1b", 16384, BF16, [2048])
            for tt in range(8):
                zk = [("z", tt, db) for db in range(4)]
                S.op("act", lambda e, tt=tt: e.activation(h1b, z[:, tt, :], AF.Copy, scale=1.0 / ALPHA), reads=zk, writes=["h1b"])
                for c_ in range(2):
                    def fsc(eng, tt=tt, c_=c_):
                        return eng.indirect_dma_start(out=Xslots[:, :], out_offset=bass.IndirectOffsetOnAxis(ap=sl_i[:, tt, c_:c_ + 1], axis=0),
                                                      in_=h1b, in_offset=None)
                    S.dma_fn("pool", fsc, reads=["h1b", "sl_i", "Xzero"], writes=[("Xslots", tt, c_)])
            xsk = [("Xslots", tt, c_) for tt in range(8) for c_ in range(2)]
            if stage == "S1":
                S.dma("sp", xs[0], Xslots[0:128, :], reads=xsk, writes=["xs0"])
                dump("dbg_xs", xs[0], [128, 2048], BF16, ["xs0"])
                dump("dbg_sli", sl_i, [128, 8, 2], I32, ["sl_i"])
                S.emit(final_wait_keys=outs)
                return nc
            lng2_loaded = False
            bndbox = {}
            ps0b = ps[0][:, :].bitcast(BF16)
            ps1b = ps[1][:, :].bitcast(BF16)
            ps3b = ps[3][:, :].bitcast(BF16)
            def gather_w(j, which):
                if which == 0:
                    b = j % 2

                    def fw1(eng):
                        if "r" not in bndbox:
                            r_ = eng.alloc_register("moe_bnd")
                            eng.reg_mov(r_, 32 * 128 - 1)
                            bndbox["r"] = r_
                        return eng.indirect_dma_start(out=wgu[b].rearrange("p a b -> p (a b)"), out_offset=None,
                                                      in_=w_gu.rearrange("e p n -> (e p) n"),
                                                      in_offset=bass.IndirectOffsetOnAxis(ap=ej_i[:, j:j + 1], axis=0),
                                                      bounds_check=bndbox["r"], oob_is_err=False)
                    S.dma_fn("pool", fw1, reads=["ej_i"], writes=[("wgu%d" % b, 0), ("wgu%d" % b, 1)])
                else:
                    b3 = j % 3

                    def fw2(eng):
                        return eng.indirect_dma_start(out=wdn[b3].rearrange("p a b -> p (a b)"), out_offset=None,
                                                      in_=w_dn.rearrange("e p n -> (e p) n"),
                                                      in_offset=bass.IndirectOffsetOnAxis(ap=ej_i[:, j:j + 1], axis=0),
                                                      bounds_check=bndbox["r"], oob_is_err=False)
                    S.dma_fn("pool", fw2, reads=["ej_i"], writes=["wdn%d" % b3])

            def stage_a1(j):
                b = j % 2
                xb = xs[b]
                S.dma("sp", xb, Xslots[j * 128:(j + 1) * 128, :], reads=xsk, writes=["xs%d" % b])

                def ft1(e):
                    for c in range(8):
                        ins = e.transpose(ps0b[:, c * 128:(c + 1) * 128], xb[:, c * 128:(c + 1) * 128], ident_b[:, :])
                    return ins

                def ft2(e):
                    for c in range(8, 16):
                        ins = e.transpose(ps1b[:, (c - 8) * 128:(c - 7) * 128], xb[:, c * 128:(c + 1) * 128], ident_b[:, :])
                    return ins
                S.op("pe", ft1, reads=["xs%d" % b, "ident_b"], writes=[PK[0]])
                S.op("pe", ft2, reads=["xs%d" % b, "ident_b"], writes=[PK[1]])
                xT = xsT[b]
                S.op("dve", lambda e: e.tensor_copy(xT[:, 0:8, :].rearrange("p a b -> p (a b)"), ps0b[:, 0:1024]), reads=[PK[0]], writes=[("xsT%d" % b, 0)])
                S.op("act", lambda e: e.activation(xT[:, 8:16, :].rearrange("p a b -> p (a b)"), ps1b[:, 0:1024], AF.Copy), reads=[PK[1]], writes=[("xsT%d" % b, 1)])

            def stage_a2(j):
                b = j % 2
                r3 = j % 3
                xT = xsT[b]

                def fg(e):
                    for c in range(16):
                        ins = e.matmul(ps[2][:, :], xT[:, c, :], wgu[b][:, c, :], start=(c == 0), stop=(c == 15))
                    return ins
                S.op("pe", fg, reads=[("xsT%d" % b, 0), ("xsT%d" % b, 1), ("wgu%d" % b, 0), ("wgu%d" % b, 1)], writes=[PK[2]])
                S.op("act", lambda e: e.activation(sgt1[r3], ps[2][:, 0:256], AF.Silu), reads=[PK[2]], writes=["sgt1_%d" % r3])
                S.op("dve", lambda e: e.tensor_tensor(actb1[r3], sgt1[r3], ps[2][:, 256:512], ALU.mult), reads=["sgt1_%d" % r3, PK[2]], writes=["actb1_%d" % r3])

            def stage_b(j):
                r3 = j % 3

                def fta(e):
                    for fc in range(2):
                        ins = e.transpose(ps3b[:, fc * 128:(fc + 1) * 128], actb1[r3][:, fc * 128:(fc + 1) * 128], ident_b[:, :])
                    return ins
                S.op("pe", fta, reads=["actb1_%d" % r3, "ident_b"], writes=[PK[3]])
                S.op("act", lambda e: e.activation(actT1[r3].rearrange("p a b -> p (a b)"), ps3b[:, 0:256], AF.Copy), reads=[PK[3]], writes=["actT1_%d" % r3])

            def stage_c(j):
                r3 = j % 3
                b3 = j % 3
                yb = j % 2
                y_ = ys[yb]

                def fd(e):
                    for db in range(4):
                        for fc in range(2):
                            ins = e.matmul(ps[4 + db][:, :], actT1[r3][:, fc, :], wdn[b3][:, fc, db * 512:(db + 1) * 512],
                                           start=(fc == 0), stop=(fc == 1))
                    return ins
                S.op("pe", fd, reads=["actT1_%d" % r3, "wdn%d" % b3], writes=[PK[4], PK[5], PK[6], PK[7]])
                for db in range(4):
                    if db % 2 == 0:
                        S.op("dve", lambda e, db=db: e.tensor_copy(y_[:, db * 512:(db + 1) * 512], ps[4 + db][:, :]), reads=[PK[4 + db]], writes=[("ys%d" % yb, db)])
                    else:
                        S.op("act", lambda e, db=db: e.activation(y_[:, db * 512:(db + 1) * 512], ps[4 + db][:, :], AF.Copy), reads=[PK[4 + db]], writes=[("ys%d" % yb, db)])
                S.dma("sp", Yslots[j * 128:(j + 1) * 128, :], y_, reads=[("ys%d" % yb, db) for db in range(4)], writes=[("Yslots", j)])

            gather_w(0, 0)
            gather_w(0, 1)
            for s_ in range(NSL + 2):
                if s_ + 1 < NSL:
                    gather_w(s_ + 1, 0)
                if s_ < NSL:
                    stage_a1(s_)
                if 1 <= s_ <= NSL:
                    stage_b(s_ - 1)
                if s_ >= 2:
                    stage_c(s_ - 2)
                if s_ < NSL:
                    stage_a2(s_)
                if s_ + 1 < NSL:
                    gather_w(s_ + 1, 1)
            lng2 = AR.alloc("lnDg", 65536, F32, [2048])
            lnb2 = AR.alloc("lnDb", 73728, F32, [2048])
            yg = [HA.alloc("yg0", 0, BF16, [2048]), HA.alloc("yg1", 4096, BF16, [2048])]
            ysk = [("Yslots", j) for j in range(NSL)]
            for tt in range(8):
                for c_ in range(2):
                    def fga(eng, tt=tt, c_=c_):
                        return eng.indirect_dma_start(out=yg[c_], out_offset=None, in_=Yslots[:, :],
                                                      in_offset=bass.IndirectOffsetOnAxis(ap=sl_i[:, tt, c_:c_ + 1], axis=0))
                    S.dma_fn("pool", fga, reads=ysk + ["sl_i"], writes=["yg%d" % c_])
                    S.op("dve", lambda e, tt=tt, c_=c_: e.scalar_tensor_tensor(z[:, tt, :], yg[c_], W12[:, tt, c_:c_ + 1], z[:, tt, :], ALU.mult, ALU.add),
                         reads=["yg%d" % c_, ("W1", tt), ("W2", tt)] + [("z", tt, db) for db in range(4)], writes=[("z", tt, db) for db in range(4)])

        else:
            wgu = [AR.alloc("wgu0", 65536, BF16, [16, 512]), AR.alloc("wgu1", 90112, BF16, [16, 512])]
            wdn = [AR.alloc("wdn0", 81920, BF16, [2, 2048]), AR.alloc("wdn1", 106496, BF16, [2, 2048])]
            lng2 = AR.alloc("lnDg", 114688, F32, [2048])
            lnb2 = AR.alloc("lnDb", 122880, F32, [2048])
            sgt = [QA.alloc("sgt%d" % k, 4096 + k * 1024, F32, [256]) for k in range(3)]
            actb = [QA.alloc("actb%d" % k, 8192 + k * 512, BF16, [256]) for k in range(3)]
            actT = [QA.alloc("actT%d" % k, 10240 + k * 512, BF16, [2, 128]) for k in range(3)]
            stD = QA.alloc("stD", 12288, F32, [64])

            N = 32 * 8
            ps2b = ps[2][:, :].bitcast(BF16)

            def load_expert(ex):
                b = ex % 2
                S.dma("pool", wgu[b].rearrange("p a b -> p (a b)"), w_gu[ex], writes=[("wgu%d" % b, 0), ("wgu%d" % b, 1)])
                S.dma("pool", wdn[b].rearrange("p a b -> p (a b)"), w_dn[ex], writes=["wdn%d" % b])

            load_expert(0)
            for step in range(N + 2):
                if step < N:
                    k = step
                    ex, tt = divmod(k, 8)
                    b = ex % 2
                    gb = k % 2
                    r3 = k % 3

                    def f(e, b=b, tt=tt, gb=gb):
                        for c in range(16):
                            ins = e.matmul(ps[gb][:, :], h1T[:, c, tt * 128:(tt + 1) * 128], wgu[b][:, c, :],
                                           start=(c == 0), stop=(c == 15))
                        return ins
                    S.op("pe", f, reads=[("wgu%d" % b, 0), ("wgu%d" % b, 1)] + [("h1T", c4, tt) for c4 in range(4)], writes=[PK[gb]])
                    S.op("act", lambda e, r3=r3, gb=gb: e.activation(sgt[r3], ps[gb][:, 0:256], AF.Silu),
                         reads=[PK[gb]], writes=["sgt%d" % r3])
                    S.op("dve", lambda e, r3=r3, gb=gb, tt=tt, ex=ex: e.scalar_tensor_tensor(
                        actb[r3], sgt[r3], comb[:, tt, ex:ex + 1], ps[gb][:, 256:512], ALU.mult, ALU.mult),
                        reads=["sgt%d" % r3, PK[gb], ("comb", tt)], writes=["actb%d" % r3])
                if 1 <= step <= N:
                    k = step - 1
                    r3 = k % 3

                    def ft(e, r3=r3):
                        for fc in range(2):
                            ins = e.transpose(ps2b[:, fc * 128:(fc + 1) * 128], actb[r3][:, fc * 128:(fc + 1) * 128], ident_b[:, :])
                        return ins
                    S.op("pe", ft, reads=["actb%d" % r3, "ident_b"], writes=[PK[2]])
                    S.op("act", lambda e, r3=r3: e.activation(actT[r3].rearrange("p a b -> p (a b)"), ps2b[:, 0:256], AF.Copy),
                         reads=[PK[2]], writes=["actT%d" % r3])
                if step >= 2:
                    k = step - 2
                    ex, tt = divmod(k, 8)
                    b = ex % 2
                    r3 = k % 3

                    def fd(e, b=b, r3=r3):
                        for db in range(4):
                            for fc in range(2):
                                ins = e.matmul(ps[4 + db][:, :], actT[r3][:, fc, :], wdn[b][:, fc, db * 512:(db + 1) * 512],
                                               start=(fc == 0), stop=(fc == 1))
                        return ins
                    S.op("pe", fd, reads=["actT%d" % r3, "wdn%d" % b], writes=[PK[4], PK[5], PK[6], PK[7]])
                    for db in range(4):
                        S.op("dve", lambda e, tt=tt, db=db: e.tensor_tensor(
                            z[:, tt, db * 512:(db + 1) * 512], z[:, tt, db * 512:(db + 1) * 512], ps[4 + db][:, :], ALU.add),
                            reads=[PK[4 + db], ("z", tt, db)], writes=[("z", tt, db)])
                if step % 8 == 1 and step // 8 + 1 < 32:
                    load_expert(step // 8 + 1)


        S.dma("sp", lng2, ln2g_d, writes=["lnDg"])
        S.dma("sp", lnb2, ln2b_d, writes=["lnDb"])
        out = dout("out", [NT, D], F32)

        stD2 = [stD, QA.alloc("stD1", 12544, F32, [64])]

        def d_chain(tt):
            ch = Chain()
            zk = ln_stages(ch, tt, stD2[tt % 2], lng2, lnb2, "lnDg", "lnDb", "_d%d" % (tt % 2))

            def fin():
                S.dma("sp", out[tt * 128:(tt + 1) * 128, :], z[:, tt, :], reads=zk, writes=[("out", tt)])
                outs.append(("out", tt))
            ch.st.append(fin)
            return ch.st

        interleave([d_chain(tt) for tt in range(8)], 2)
        S.emit(final_wait_keys=outs)
    return nc


def _bucket_table():
    rel = np.arange(-(SEQ - 1), SEQ, dtype=np.int32)
    try:
        import jax
        import jax.numpy as jnp
        cpu = jax.devices("cpu")[0]
        with jax.default_device(cpu):
            r = jnp.asarray(rel)
            half, max_exact = 16, 8
            ret = jnp.where(r > 0, half, 0)
            n = jnp.abs(r)
            nf = jnp.maximum(n, 1).astype(jnp.float32)
            large = max_exact + (jnp.log(nf / max_exact) / math.log(128 / max_exact) * (half - max_exact)).astype(jnp.int32)
            large = jnp.minimum(large, half - 1)
            b = np.asarray(ret + jnp.where(n < max_exact, n, large))
    except Exception:
        half, max_exact = 16, 8
        ret = np.where(rel > 0, half, 0)
        n = np.abs(rel)
        nf = np.maximum(n, 1).astype(np.float32)
        large = max_exact + (np.log(nf / np.float32(max_exact)) / np.float32(math.log(128 / max_exact)) * np.float32(half - max_exact)).astype(np.int32)
        large = np.minimum(large, half - 1)
        b = ret + np.where(n < max_exact, n, large)
    return {int(r): int(v) for r, v in zip(rel, b)}


def _etables(r, bt):
    E1 = np.zeros((32, NTYPES, 256), np.float32)
    E2 = np.zeros((32, 26), np.float32)

    def toep(ty, Dblk):
        for i in range(255):
            E1[bt[128 * Dblk + 127 - i], ty, i] = 1.0

    def const(ty, b):
        E1[b, ty, :255] = 1.0
    toep(0, 0)
    toep(1, 1)
    toep(2, -1)
    const(3, 31)
    const(4, 15)
    if r < 3:
        toep(5, 1)
        const(6, 31)
    else:
        const(5, 15)
        const(6, 15)
    if r > 0:
        toep(7, -1)
        const(8, 15)
    else:
        const(7, 31)
        const(8, 31)
    for j in range(8, 32):
        wrapped = (8 * r + j) >= 32
        E2[15 if wrapped else 31, j - 8] = 1.0
    E2[15, 24] = 1.0
    E2[31, 25] = 1.0
    return E1.reshape(32, NTYPES * 256), E2


_NC_CACHE = {}


def make_in_maps(inputs):
    f = lambda a: np.ascontiguousarray(np.asarray(a, dtype=np.float32))
    x = f(inputs["x"])
    bt = _bucket_table()
    rep = lambda v, n=128: np.ascontiguousarray(np.broadcast_to(np.asarray(v, np.float32).reshape(1, -1), (n, np.asarray(v).size)))

    def ptile(w):
        K, n = w.shape
        return np.ascontiguousarray(w.reshape(K // 128, 128, n).transpose(1, 0, 2).reshape(128, (K // 128) * n))

    w_in = f(inputs["w_in"][0])
    w_out = f(inputs["w_out"][0])
    weg = f(inputs["w_exp_gate"][0])
    weu = f(inputs["w_exp_up"][0])
    wed = f(inputs["w_exp_down"][0])
    w_inA = np.stack([ptile(w_in[:, c0:c0 + 512]) for c0 in (0, 512, 3072, 3584, 4096, 4608)])
    w_kv = np.stack([np.stack([ptile(w_in[:, 1024 + hg * 256:1024 + (hg + 1) * 256]),
                               ptile(w_in[:, 2048 + hg * 256:2048 + (hg + 1) * 256])]) for hg in range(4)])
    w_outT = np.stack([ptile(w_out[:, db * 512:(db + 1) * 512]) for db in range(4)])
    w_gu = np.stack([ptile(np.concatenate([weg[e], weu[e]], axis=1)) for e in range(32)])
    w_dn = np.stack([ptile(wed[e]) for e in range(32)])
    wr_cat = np.ascontiguousarray(np.concatenate(
        [f(inputs["w_router_group"][0])] + [f(inputs["w_router_expert"][0][g]) for g in range(4)], axis=1))
    br = np.concatenate([f(inputs["b_router_group"][0]).reshape(-1), f(inputs["b_router_expert"][0]).reshape(-1)])
    lamp = np.concatenate([f(inputs[k][0]).reshape(-1) for k in ("lam_q1", "lam_k1", "lam_q2", "lam_k2")])
    common = {
        "w_inA": w_inA, "w_kv": w_kv, "w_outT": w_outT, "w_gu": w_gu, "w_dn": w_dn,
        "wr_cat": wr_cat, "br_b": rep(br),
        "ln1g_b": rep(inputs["ln1_g"][0]), "ln1b_b": rep(inputs["ln1_b"][0]),
        "ln2g_b": rep(inputs["ln2_g"][0]), "ln2b_b": rep(inputs["ln2_b"][0]),
        "rel_bias": f(inputs["rel_bias"]), "lamp_b": rep(lamp), "subg_b": rep(inputs["subln_g"][0]),
        "sglng_b": rep(f(inputs["sg_ln_g"][0]).reshape(-1)), "sglnb_b": rep(f(inputs["sg_ln_b"][0]).reshape(-1)),
        "sg_wT": np.ascontiguousarray(f(inputs["sg_w"][0]).transpose(0, 2, 1)),
        "sg_b_row": f(inputs["sg_b"][0]).reshape(1, 1024),
        "ident": np.eye(128, dtype=np.float32),
    }
    if MOE_SPARSE:
        common["tri"] = np.ascontiguousarray(np.triu(np.ones((128, 128), np.float32), 1))
        common["iota_p"] = np.arange(128, dtype=np.float32).reshape(128, 1)
    in_maps = []
    for c in range(8):
        b, r = divmod(c, 4)
        idx = np.concatenate([((8 * r + j) % 32) * 128 + (127 - np.arange(128)) for j in range(32)])
        E1, E2 = _etables(r, bt)
        m = dict(common)
        xTa = x[b][idx].T
        m["xT_all"] = np.ascontiguousarray(xTa.reshape(16, 128, 16, 256).transpose(2, 1, 0, 3).reshape(16, 128, 4096))
        m["xT_own"] = ptile(np.ascontiguousarray(x[b, r * NT:(r + 1) * NT].T))
        m["x_own"] = np.ascontiguousarray(x[b, r * NT:(r + 1) * NT])
        m["E1"] = E1
        m["E2"] = E2
        in_maps.append(m)
    return in_maps


def kernel(**inputs):
    if "nc" not in _NC_CACHE:
        _NC_CACHE["nc"] = build("D")
    nc = _NC_CACHE["nc"]
    in_maps = make_in_maps(inputs)
    res = run_bass_kernel_spmd(nc, in_maps, core_ids=list(range(8)))
    out = np.zeros((2, SEQ, D), np.float32)
    for c in range(8):
        b, r = divmod(c, 4)
        out[b, r * NT:(r + 1) * NT] = res.results[c]["out"]
    return out
```
